# Optimizing a Trainium2 kernel written in Bass

```python
import jax, jax.numpy as jnp
from jax import lax
import numpy as np

D_MODEL = 2048
BATCH = 8
SEQ = 2048
DEPTH = 1

D_MIX = D_MODEL
D_ATT = D_MIX // 2
D_RNN = D_MIX - D_ATT
N_Q_HEADS = 16
N_KV_GROUPS = 4
HEADS_PER_GROUP = N_Q_HEADS // N_KV_GROUPS
HEAD_DIM = D_ATT // N_Q_HEADS
D_KV = N_KV_GROUPS * HEAD_DIM
CMP_BLOCK = 32
CMP_STRIDE = 16
CMP_HIDDEN = 4 * HEAD_DIM
SEL_BLOCK = 64
SEL_TOP_N = 8
WINDOW = 512
Q_CHUNK = 128
N_BRANCH = 3
RNN_BLOCKS = 8
RNN_BLOCK_DIM = D_RNN // RNN_BLOCKS
CONV_WIDTH = 4
LRU_C = 8.0
D_FF = 4 * D_MODEL
EPS = 1e-6
NEG = -1e30
FORCE_SCORE = 1e9

SPLIT_SIZES = [D_ATT] + [D_KV] * 6 + [N_BRANCH * N_Q_HEADS, D_RNN, D_RNN]
D_IN = sum(SPLIT_SIZES)
SPLIT_POINTS = [int(v) for v in np.cumsum(SPLIT_SIZES)[:-1]]

kernel_name = "hymba_nsa_rglru_sandwich_adaln_block"


def rms_norm(x, g):
    xf = x.astype(jnp.float32)
    y = xf * lax.rsqrt(jnp.mean(xf * xf, axis=-1, keepdims=True) + EPS)
    return (y * g.astype(jnp.float32)).astype(x.dtype)


def masked_softmax(s, mask):
    p = jax.nn.softmax(jnp.where(mask, s.astype(jnp.float32), NEG), axis=-1)
    return jnp.where(mask, p, 0.0)


def compress_kv(k, w1, w2, pe):
    B, S = k.shape[0], k.shape[1]
    n_cmp = (S - CMP_BLOCK) // CMP_STRIDE + 1
    idx = jnp.arange(n_cmp)[:, None] * CMP_STRIDE + jnp.arange(CMP_BLOCK)[None, :]
    blk = k[:, idx] + pe[None, None, :, None, :]
    blk = jnp.moveaxis(blk, 2, 3).reshape(B, n_cmp, N_KV_GROUPS, CMP_BLOCK * HEAD_DIM)
    return jax.nn.gelu(blk @ w1) @ w2


def cmp_to_sel_weights(n_cmp, n_sel):
    c0 = np.arange(n_cmp)[:, None] * CMP_STRIDE
    s0 = np.arange(n_sel)[None, :] * SEL_BLOCK
    ov = np.minimum(c0 + CMP_BLOCK, s0 + SEL_BLOCK) - np.maximum(c0, s0)
    return jnp.asarray(np.clip(ov, 0, None).astype(np.float32) / CMP_BLOCK)


def nsa_attention(q, kc, vc, ks, vs, kw, vw, gate_logits, w1k, w2k, pek, w1v, w2v, pev):
    B, S = q.shape[0], q.shape[1]
    G, Hg, Dh = N_KV_GROUPS, HEADS_PER_GROUP, HEAD_DIM
    q = q.reshape(B, S, G, Hg, Dh) * (Dh ** -0.5)
    kc, vc, ks, vs, kw, vw = [t.reshape(B, S, G, Dh) for t in (kc, vc, ks, vs, kw, vw)]
    pos = jnp.arange(S)

    k_cmp = compress_kv(kc, w1k, w2k, pek)
    v_cmp = compress_kv(vc, w1v, w2v, pev)
    n_cmp = k_cmp.shape[1]
    cmp_end = jnp.arange(n_cmp) * CMP_STRIDE + CMP_BLOCK - 1
    mask_c = (cmp_end[None, :] <= pos[:, None])[None, :, None, None, :]
    s_c = jnp.einsum('bsghd,bngd->bsghn', q, k_cmp)
    p_c = masked_softmax(s_c, mask_c)
    o_cmp = jnp.einsum('bsghn,bngd->bsghd', p_c.astype(v_cmp.dtype), v_cmp)

    n_sel = S // SEL_BLOCK
    imp = jnp.einsum('bsghn,nj->bsgj', p_c, cmp_to_sel_weights(n_cmp, n_sel))
    blk = jnp.arange(n_sel)[None, :]
    cur = (pos // SEL_BLOCK)[:, None]
    forced = (blk == 0) | (blk == cur) | (blk == cur - 1)
    valid = blk * SEL_BLOCK <= pos[:, None]
    imp = jnp.where(forced[None, :, None, :], FORCE_SCORE,
                    jnp.where(valid[None, :, None, :], imp, -FORCE_SCORE))
    top_n = min(SEL_TOP_N, n_sel)
    _, sel_idx = lax.top_k(imp, top_n)

    ks_blk = ks.reshape(B, n_sel, SEL_BLOCK, G, Dh).transpose(0, 3, 1, 2, 4)
    vs_blk = vs.reshape(B, n_sel, SEL_BLOCK, G, Dh).transpose(0, 3, 1, 2, 4)
    kw_pad = jnp.pad(kw, ((0, 0), (WINDOW, 0), (0, 0), (0, 0)))
    vw_pad = jnp.pad(vw, ((0, 0), (WINDOW, 0), (0, 0), (0, 0)))
    b_ix = jnp.arange(B)[:, None, None, None]
    g_ix = jnp.arange(G)[None, None, :, None]
    n_keys_sel = top_n * SEL_BLOCK

    def query_block(ci):
        start = ci * Q_CHUNK
        tq = start + jnp.arange(Q_CHUNK)
        qc = lax.dynamic_slice_in_dim(q, start, Q_CHUNK, axis=1)
        idx = lax.dynamic_slice_in_dim(sel_idx, start, Q_CHUNK, axis=1)
        k_sel = ks_blk[b_ix, g_ix, idx].reshape(B, Q_CHUNK, G, n_keys_sel, Dh)
        v_sel = vs_blk[b_ix, g_ix, idx].reshape(B, Q_CHUNK, G, n_keys_sel, Dh)
        kpos = (idx[..., None] * SEL_BLOCK + jnp.arange(SEL_BLOCK)).reshape(B, Q_CHUNK, G, n_keys_sel)
        mask_s = (kpos <= tq[None, :, None, None])[:, :, :, None, :]
        p_s = masked_softmax(jnp.einsum('bcghd,bcgnd->bcghn', qc, k_sel), mask_s)
        o_s = jnp.einsum('bcghn,bcgnd->bcghd', p_s.astype(v_sel.dtype), v_sel)
        k_w = lax.dynamic_slice_in_dim(kw_pad, start, Q_CHUNK + WINDOW, axis=1)
        v_w = lax.dynamic_slice_in_dim(vw_pad, start, Q_CHUNK + WINDOW, axis=1)
        wpos = start - WINDOW + jnp.arange(Q_CHUNK + WINDOW)
        mask_w = ((wpos[None, :] <= tq[:, None]) & (wpos[None, :] > tq[:, None] - WINDOW)
                  & (wpos[None, :] >= 0))[None, :, None, None, :]
        p_w = masked_softmax(jnp.einsum('bcghd,bkgd->bcghk', qc, k_w), mask_w)
        o_w = jnp.einsum('bcghk,bkgd->bcghd', p_w.astype(v_w.dtype), v_w)
        return o_s, o_w

    o_sel, o_win = lax.map(query_block, jnp.arange(S // Q_CHUNK))
    o_sel = jnp.moveaxis(o_sel, 0, 1).reshape(B, S, G, Hg, Dh)
    o_win = jnp.moveaxis(o_win, 0, 1).reshape(B, S, G, Hg, Dh)

    g = jax.nn.sigmoid(gate_logits).reshape(B, S, G, Hg, N_BRANCH)
    o = g[..., 0:1] * o_cmp + g[..., 1:2] * o_sel + g[..., 2:3] * o_win
    return o.reshape(B, S, D_ATT)


def causal_depthwise_conv(x, w, b):
    S = x.shape[1]
    xp = jnp.pad(x, ((0, 0), (CONV_WIDTH - 1, 0), (0, 0)))
    return b + sum(xp[:, k:k + S] * w[k] for k in range(CONV_WIDTH))


def block_diag_linear(x, w, b):
    B, S = x.shape[0], x.shape[1]
    xb = x.reshape(B, S, RNN_BLOCKS, RNN_BLOCK_DIM)
    return jnp.einsum('bsni,nij->bsnj', xb, w).reshape(B, S, D_RNN) + b


def rg_lru(x, w_a, b_a, w_x, b_x, lam):
    xf = x.astype(jnp.float32)
    r = jax.nn.sigmoid(block_diag_linear(x, w_a, b_a).astype(jnp.float32))
    i = jax.nn.sigmoid(block_diag_linear(x, w_x, b_x).astype(jnp.float32))
    log_a = -LRU_C * r * jax.nn.softplus(-lam.astype(jnp.float32))
    a = jnp.exp(log_a)
    bterm = jnp.sqrt(-jnp.expm1(2.0 * log_a)) * (i * xf)

    def combine(lhs, rhs):
        a1, b1 = lhs
        a2, b2 = rhs
        return a1 * a2, a2 * b1 + b2

    _, h = lax.associative_scan(combine, (a, bterm), axis=1)
    return h.astype(x.dtype)


def setup_inputs(seed: int = 0) -> dict:
    key = jax.random.key(seed)
    ks = jax.random.split(key, 32)
    nrm = lambda k, shape, s: jax.random.normal(k, shape, jnp.float32) * s
    L = DEPTH
    u = jax.random.uniform(ks[21], (L, D_RNN), jnp.float32, 0.9, 0.999)
    a0 = u ** (1.0 / LRU_C)
    return {
        "x": nrm(ks[0], (BATCH, SEQ, D_MODEL), 1.0),
        "c": nrm(ks[1], (BATCH, D_MODEL), 1.0),
        "w_ada": nrm(ks[2], (L, D_MODEL, 6 * D_MODEL), D_MODEL ** -0.5),
        "b_ada": nrm(ks[3], (L, 6 * D_MODEL), 0.01),
        "g_pre_mix": 1.0 + nrm(ks[4], (L, D_MODEL), 0.01),
        "g_post_mix": 1.0 + nrm(ks[5], (L, D_MODEL), 0.01),
        "g_pre_mlp": 1.0 + nrm(ks[6], (L, D_MODEL), 0.01),
        "g_post_mlp": 1.0 + nrm(ks[7], (L, D_MODEL), 0.01),
        "w_in": nrm(ks[8], (L, D_MODEL, D_IN), D_MODEL ** -0.5),
        "cmp_w1_k": nrm(ks[9], (L, CMP_BLOCK * HEAD_DIM, CMP_HIDDEN), (CMP_BLOCK * HEAD_DIM) ** -0.5),
        "cmp_w2_k": nrm(ks[10], (L, CMP_HIDDEN, HEAD_DIM), CMP_HIDDEN ** -0.5),
        "cmp_pe_k": nrm(ks[11], (L, CMP_BLOCK, HEAD_DIM), 0.1),
        "cmp_w1_v": nrm(ks[12], (L, CMP_BLOCK * HEAD_DIM, CMP_HIDDEN), (CMP_BLOCK * HEAD_DIM) ** -0.5),
        "cmp_w2_v": nrm(ks[13], (L, CMP_HIDDEN, HEAD_DIM), CMP_HIDDEN ** -0.5),
        "cmp_pe_v": nrm(ks[14], (L, CMP_BLOCK, HEAD_DIM), 0.1),
        "conv_w": nrm(ks[15], (L, CONV_WIDTH, D_RNN), CONV_WIDTH ** -0.5),
        "conv_b": nrm(ks[16], (L, D_RNN), 0.01),
        "w_rg_a": nrm(ks[17], (L, RNN_BLOCKS, RNN_BLOCK_DIM, RNN_BLOCK_DIM), RNN_BLOCK_DIM ** -0.5),
        "b_rg_a": nrm(ks[18], (L, D_RNN), 0.01),
        "w_rg_x": nrm(ks[19], (L, RNN_BLOCKS, RNN_BLOCK_DIM, RNN_BLOCK_DIM), RNN_BLOCK_DIM ** -0.5),
        "b_rg_x": nrm(ks[20], (L, D_RNN), 0.01),
        "lru_lambda": jnp.log(a0) - jnp.log1p(-a0),
        "g_grp_att": 1.0 + nrm(ks[22], (L, D_ATT), 0.01),
        "g_grp_rnn": 1.0 + nrm(ks[23], (L, D_RNN), 0.01),
        "w_out": nrm(ks[24], (L, D_MIX, D_MODEL), D_MIX ** -0.5),
        "w_ff1": nrm(ks[25], (L, D_MODEL, D_FF), D_MODEL ** -0.5),
        "w_ff2": nrm(ks[26], (L, D_FF, D_MODEL), D_FF ** -0.5),
    }


def reference(x, c, w_ada, b_ada, g_pre_mix, g_post_mix, g_pre_mlp, g_post_mlp, w_in,
              cmp_w1_k, cmp_w2_k, cmp_pe_k, cmp_w1_v, cmp_w2_v, cmp_pe_v,
              conv_w, conv_b, w_rg_a, b_rg_a, w_rg_x, b_rg_x, lru_lambda,
              g_grp_att, g_grp_rnn, w_out, w_ff1, w_ff2):
    c_act = jax.nn.silu(c)
    for l in range(DEPTH):
        mod = c_act @ w_ada[l] + b_ada[l]
        sh1, sc1, gt1, sh2, sc2, gt2 = [m[:, None, :] for m in jnp.split(mod, 6, axis=-1)]

        h = rms_norm(x, g_pre_mix[l]) * (1.0 + sc1) + sh1
        proj = h @ w_in[l]
        q, kc, vc, ksl, vsl, kw, vw, gl, xr, yr = jnp.split(proj, SPLIT_POINTS, axis=-1)
        o_att = nsa_attention(q, kc, vc, ksl, vsl, kw, vw, gl,
                              cmp_w1_k[l], cmp_w2_k[l], cmp_pe_k[l],
                              cmp_w1_v[l], cmp_w2_v[l], cmp_pe_v[l])
        xr = causal_depthwise_conv(xr, conv_w[l], conv_b[l])
        hr = rg_lru(xr, w_rg_a[l], b_rg_a[l], w_rg_x[l], b_rg_x[l], lru_lambda[l])
        o_rnn = jax.nn.gelu(yr) * hr
        mix = jnp.concatenate([rms_norm(o_att, g_grp_att[l]),
                               rms_norm(o_rnn, g_grp_rnn[l])], axis=-1) @ w_out[l]
        x = x + gt1 * rms_norm(mix, g_post_mix[l])

        h = rms_norm(x, g_pre_mlp[l]) * (1.0 + sc2) + sh2
        f = jnp.square(jax.nn.relu(h @ w_ff1[l])) @ w_ff2[l]
        x = x + gt2 * rms_norm(f, g_post_mlp[l])
    return x
```

```python
import numpy as np
import concourse.bass as bass
import concourse.mybir as mybir
from concourse.bass_utils import run_bass_kernel_spmd

F32 = mybir.dt.float32
BF16 = mybir.dt.bfloat16
U8 = mybir.dt.uint8
AF = mybir.ActivationFunctionType
ALU = mybir.AluOpType
AX = mybir.AxisListType
ESZ = {F32: 4, BF16: 2, U8: 1, mybir.dt.int32: 4, mybir.dt.uint32: 4}
PAGE = 2048


def region(ap):
    t = ap.tensor
    sp = str(ap.space)
    if 'DRAM' in sp.upper() or 'HBM' in sp.upper() or type(t).__name__.startswith('DRam'):
        return None
    esz = ESZ[ap.dtype]
    rowlen = 1
    for s in list(t.shape)[1:]:
        rowlen *= int(s)
    pairs = [(int(a), int(b)) for a, b in ap.ap]
    off = int(ap.offset)
    p_lo = off // rowlen
    col = off % rowlen
    pstep, pcnt = pairs[0]
    if pstep == 0:
        pcnt = 1
    ext = 1
    for st, cnt in pairs[1:]:
        ext += (cnt - 1) * abs(st)
    lo, hi = col * esz, (col + ext) * esz
    if 'PSum' in type(t).__name__:
        return (t.name, 0, 128, lo // 2048 * 2048, ((hi - 1) // 2048 + 1) * 2048)
    return (t.name, p_lo, p_lo + pcnt, lo, hi)


def overlap(a, b):
    return a[0] == b[0] and a[1] < b[2] and b[1] < a[2] and a[3] < b[4] and b[3] < a[4]


def contains(outer, inner):
    return (outer[0] == inner[0] and outer[1] <= inner[1] and inner[2] <= outer[2]
            and outer[3] <= inner[3] and inner[4] <= outer[4])


class Op:
    __slots__ = ('eng', 'fn', 'deps', 'signal', 'need', 'is_dma', 'idx', 'prev_dma', 'chain')

    def __init__(self, eng, fn, is_dma=False):
        self.eng = eng
        self.fn = fn
        self.deps = set()
        self.signal = None
        self.need = False
        self.is_dma = is_dma
        self.prev_dma = None
        self.chain = eng


class Prog:
    ENGS = ('pe', 'act', 'dve', 'pool', 'sp')
    NDSEM = 8

    def __init__(self, nc):
        self.nc = nc
        self.ops = {e: [] for e in self.ENGS}
        self.wrecs = {}
        self.rrecs = {}
        self.all_ops = []
        self.out_dmas = []
        self.bg_hook = None

    def _pages(self, r):
        return range(r[3] // PAGE, (r[4] - 1) // PAGE + 1)

    def _add_deps(self, op, reads, writes):
        for r in reads:
            for pg in self._pages(r):
                for (reg, o) in self.wrecs.get((r[0], pg), ()):
                    if o is not op and overlap(r, reg):
                        op.deps.add(o)
        for w in writes:
            for pg in self._pages(w):
                key = (w[0], pg)
                for (reg, o) in self.wrecs.get(key, ()):
                    if o is not op and overlap(w, reg):
                        op.deps.add(o)
                for (reg, o) in self.rrecs.get(key, ()):
                    if o is not op and overlap(w, reg):
                        op.deps.add(o)
        for w in writes:
            for pg in self._pages(w):
                key = (w[0], pg)
                if key in self.wrecs:
                    self.wrecs[key] = [x for x in self.wrecs[key] if not contains(w, x[0])]
                if key in self.rrecs:
                    self.rrecs[key] = [x for x in self.rrecs[key] if not contains(w, x[0])]
                self.wrecs.setdefault(key, []).append((w, op))
        for r in reads:
            for pg in self._pages(r):
                key = (r[0], pg)
                lst = self.rrecs.get(key)
                if lst:
                    self.rrecs[key] = [x for x in lst
                                       if not (x[1].eng == op.eng and not x[1].is_dma and not op.is_dma
                                               and contains(r, x[0]))]
                self.rrecs.setdefault(key, []).append((r, op))

    def op(self, eng, fn, outs=(), ins=(), is_dma=False):
        o = Op(eng, fn, is_dma)
        reads = [x for x in (region(a) for a in ins if a is not None) if x is not None]
        writes = [x for x in (region(a) for a in outs if a is not None) if x is not None]
        self._add_deps(o, reads, writes)
        if eng == 'pe':
            o.deps = {d for d in o.deps if not (d.eng == 'pe' and not d.is_dma)}
        self.ops[eng].append(o)
        self.all_ops.append(o)
        return o

    def dma(self, out, in_, q='sp', is_output=False, chain=None, deps=()):
        o = self.op(q, lambda e: e.dma_start(out=out, in_=in_), outs=[out], ins=[in_], is_dma=True)
        if chain is not None:
            o.chain = chain
        o.deps.update(deps)
        if q == 'pool' and chain is None and self.bg_hook is not None:
            self.bg_hook()
        if is_output:
            self.out_dmas.append(o)
        return o

    def matmul(self, out, lhsT, rhs, start=True, stop=True, **kw):
        return self.op('pe', lambda e: e.matmul(out, lhsT, rhs, start=start, stop=stop, **kw),
                       outs=[out], ins=[lhsT, rhs])

    def transpose(self, out, in_, ident):
        return self.op('pe', lambda e: e.transpose(out, in_, ident), outs=[out], ins=[in_, ident])

    def act(self, out, in_, func, bias=None, scale=None, accum_out=None, eng='act'):
        kw = {}
        ins = [in_]
        if bias is not None:
            kw['bias'] = bias
            if not isinstance(bias, (int, float)):
                ins.append(bias)
        if scale is not None:
            kw['scale'] = scale
            if not isinstance(scale, (int, float)):
                ins.append(scale)
        outs = [out]
        if accum_out is not None:
            kw['accum_out'] = accum_out
            outs.append(accum_out)
        return self.op(eng, lambda e: e.activation(out, in_, func, **kw), outs=outs, ins=ins)

    def ts(self, out, in0, s1, s2, op0, op1=None, eng='dve', accum_out=None):
        ins = [in0] + [s for s in (s1, s2) if s is not None and not isinstance(s, (int, float))]
        outs = [out] + ([accum_out] if accum_out is not None else [])
        kw = {}
        if accum_out is not None:
            kw['accum_out'] = accum_out
        if op1 is None:
            return self.op(eng, lambda e: e.tensor_scalar(out, in0, s1, s2, op0, **kw), outs=outs, ins=ins)
        return self.op(eng, lambda e: e.tensor_scalar(out, in0, s1, s2, op0, op1, **kw), outs=outs, ins=ins)

    def tt(self, out, in0, in1, op, eng='dve'):
        return self.op(eng, lambda e: e.tensor_tensor(out, in0, in1, op), outs=[out], ins=[in0, in1])

    def stt(self, out, in0, scalar, in1, op0, op1, eng='dve'):
        ins = [in0, in1] + ([scalar] if not isinstance(scalar, (int, float)) else [])
        return self.op(eng, lambda e: e.scalar_tensor_tensor(out, in0, scalar, in1, op0, op1),
                       outs=[out], ins=ins)

    def copy(self, out, in_, eng='dve'):
        if eng == 'act':
            return self.op('act', lambda e: e.copy(out, in_), outs=[out], ins=[in_])
        return self.op(eng, lambda e: e.tensor_copy(out, in_), outs=[out], ins=[in_])

    def memset(self, out, val, eng='dve'):
        return self.op(eng, lambda e: e.memset(out, val), outs=[out], ins=[])

    def generic(self, eng, fn, outs, ins):
        return self.op(eng, fn, outs=outs, ins=ins)

    def emit(self):
        nc = self.nc
        for e in self.ENGS:
            for i, o in enumerate(self.ops[e]):
                o.idx = i
        for o in self.all_ops:
            best = {}
            keep = set()
            for d in o.deps:
                if d.is_dma:
                    keep.add(d)
                elif d.eng not in best or best[d.eng].idx < d.idx:
                    best[d.eng] = d
            keep.update(best.values())
            o.deps = keep
            for d in keep:
                d.need = True
        fin = Op('sp', None)
        fin.deps = set(self.out_dmas)
        for d in fin.deps:
            d.need = True
        import contextlib
        with contextlib.ExitStack() as st:
            esem = {e: st.enter_context(nc.semaphore('s_' + e)) for e in self.ENGS}
            dsem = {}
            chains = sorted({o.chain for o in self.all_ops if o.is_dma})
            for q in chains:
                dsem[q] = [st.enter_context(nc.semaphore('d_%s%d' % (q, i))) for i in range(self.NDSEM)]
            dcnt = {q: [0] * self.NDSEM for q in chains}
            last = {q: [None] * self.NDSEM for q in chains}
            kk = {q: 0 for q in chains}
            for e in self.ENGS:
                cnt = 0
                for o in self.ops[e]:
                    if o.is_dma:
                        q = o.chain
                        i = kk[q] % self.NDSEM
                        kk[q] += 1
                        dcnt[q][i] += 16
                        o.signal = (dsem[q][i], dcnt[q][i])
                        o.prev_dma = last[q][i]
                        last[q][i] = o
                    elif o.need:
                        cnt += 1
                        o.signal = (esem[e], cnt)
            self.n_waits = 0
            block = st.enter_context(nc.Block())

            def run(e, eng):
                known = {}
                lst = list(self.ops[e])
                if e == 'sp':
                    lst.append(fin)
                for o in lst:
                    need = {}
                    deps = set(o.deps)
                    if o.prev_dma is not None:
                        deps.add(o.prev_dma)
                    for d in deps:
                        s, v = d.signal
                        key = id(s)
                        if key not in need or need[key][1] < v:
                            need[key] = (s, v)
                    for key, (s, v) in need.items():
                        if known.get(key, 0) < v:
                            eng.wait_ge(s, v)
                            self.n_waits += 1
                            known[key] = v
                    if o.fn is None:
                        continue
                    inst = o.fn(eng)
                    if o.is_dma:
                        inst.then_inc(o.signal[0], 16)
                    elif o.need:
                        inst.then_inc(o.signal[0], 1)

            @block.tensor
            def _(eng):
                run('pe', eng)

            @block.scalar
            def _(eng):
                run('act', eng)

            @block.vector
            def _(eng):
                run('dve', eng)

            @block.gpsimd
            def _(eng):
                run('pool', eng)

            @block.sync
            def _(eng):
                run('sp', eng)

S = 2048
D = 2048
NT = 16
QC, KCc, VCc, KSc, VSc, KWc, VWc, GLc, XRc, YRc = 0, 1024, 1280, 1536, 1792, 2048, 2304, 2560, 2608, 3632
NEGV = -30000.0
C_BADA, C_GPM, C_GPL, C_CW, C_CB, C_BA, C_BX, C_LAM, C_GRNN, C_GATT, C_C, C_PEK, C_PEV, NCOLS = \
    0, 96, 112, 128, 160, 168, 176, 184, 192, 200, 208, 224, 240, 256
K_CAUS4, K_WM, K_MASKC, K_A, K_B, K_E, K_W, NKC = 0, 2048, 6144, 8192, 8704, 9216, 11264, 11296


def build(nc, stop=99, dbg=False):
    import contextlib
    dt = lambda name, shape, kind="ExternalInput": nc.dram_tensor(name, shape, F32, kind=kind).ap()
    x_d = dt("x", [S, D])
    cols_d = dt("cols", [128, NCOLS])
    kc_d = dt("kconst", [128, NKC])
    ident_d = dt("ident", [128, 128])
    wada_d = dt("w_ada", [D, 6 * D])
    bgt_d = dt("bgt_rows", [128, 2 * D])
    gpost_d = dt("gpost_rows", [128, 2 * D])
    win_d = dt("w_in", [D, 4656])
    w1k_d = dt("cmp_w1_k", [2048, 256])
    w2k_d = dt("cmp_w2_k", [256, 64])
    w1v_d = dt("cmp_w1_v", [2048, 256])
    w2v_d = dt("cmp_w2_v", [256, 64])
    wra_d = dt("w_rg_a", [8, 128, 128])
    wrx_d = dt("w_rg_x", [8, 128, 128])
    wout_d = dt("w_out", [D, D])
    wf1_d = dt("w_ff1", [D, 4 * D])
    wf2_d = dt("w_ff2", [4 * D, D])
    out_d = dt("out", [S, D], kind="ExternalOutput")
    dbg_d = dt("dbg", [128, 8192], kind="ExternalOutput") if dbg else None
    w1s = nc.dram_tensor("w1s", [64, 128, 2048], BF16).ap()
    w2s = nc.dram_tensor("w2s", [64, 128, 2048], BF16).ap()
    wos = nc.dram_tensor("wos", [16, 128, 2048], BF16).ap()
    gm_s = nc.dram_tensor("gm_s", [2, 128, 2048], F32).ap()

    st = contextlib.ExitStack()
    sb = st.enter_context(nc.sbuf_tensor("sb", [128, 212000], U8))
    ps = st.enter_context(nc.psum_tensor("ps", [128, 4096], F32))
    P = Prog(nc)

    def A(off, nbytes, d):
        assert off + nbytes <= 212000, (off, nbytes)
        return sb[:, off:off + nbytes].bitcast(d)

    def bank(i, n=512, d=F32):
        a = ps[:, i * 512:(i + 1) * 512]
        if d is BF16:
            return a.bitcast(BF16)[:, 0:n]
        return a[:, 0:n]

    ident = A(0, 512, F32)
    identb = A(512, 256, BF16)
    cols = A(768, NCOLS * 4, F32)
    modcol = A(1792, 384, F32)
    s1col = A(2176, 64, F32)
    sh1col = A(2240, 64, F32)
    s2col = A(2304, 64, F32)
    sh2col = A(2368, 64, F32)
    cA = A(2432, 32, F32)
    cA2 = A(2464, 32, F32)
    cact = A(2496, 32, BF16)
    stat = A(2560, 256, F32)
    onesf = A(2816, 512, F32)
    pbk = A(3328, 8, F32)
    pbv = A(3336, 8, F32)
    ccf = A(3344, 64, F32)
    R1 = 4096
    R2 = R1 + 65536
    R3 = R2 + 32768
    hT = A(R1, 65536, BF16).rearrange("p (c t) -> p c t", c=16)
    orT = A(R2, 32768, BF16).rearrange("p (c t) -> p c t", c=8)

    P.dma(ident, ident_d)
    P.dma(cols, cols_d)
    P.copy(identb, ident)
    P.memset(onesf, 1.0)

    P.act(ccf, cols[:, C_C:C_C + 16], AF.Silu)
    P.copy(cact, ccf)
    wa = [A(R3 + i * 16384, 16384, BF16).rearrange("p (c n) -> p c n", c=16) for i in range(2)]
    mc_ps = bank(7, 96)
    colpieces = [0, 1, 2, 3, 4, 5, 6, 7, 12, 13, 14, 15, 16, 17, 18, 19]
    wa_k = [0]

    def ada_piece(pj):
        k = wa_k[0]
        wa_k[0] += 1
        w = wa[k % 2]
        P.dma(w, wada_d[:, pj * 512:(pj + 1) * 512].rearrange("(c p) n -> p c n", p=128), q='pool')
        for jj in range(4):
            col = pj * 4 + jj
            for fc in range(16):
                P.matmul(mc_ps[:, col:col + 1], w[:, fc, jj * 128:(jj + 1) * 128], cact[:, fc:fc + 1],
                         start=(k == 0 and jj == 0 and fc == 0), stop=True, skip_group_check=True)
    for pj in range(8):
        ada_piece(pj)
    P.tt(modcol[:, 0:32], mc_ps[:, 0:32], cols[:, C_BADA:C_BADA + 32], ALU.add)
    P.stt(s1col, modcol[:, 16:32], 1.0, cols[:, C_GPM:C_GPM + 16], ALU.add, ALU.mult)
    P.copy(sh1col, modcol[:, 0:16])
    late_pieces = [12, 13, 14, 15, 16, 17, 18, 19]

    def norm_transpose(xt_tile, dstT, tcol, scol, shcol, tmp_xn, junk, k):
        ss = stat[:, k % 8:k % 8 + 1]
        P.act(junk, xt_tile, AF.Square, accum_out=ss)
        rs = stat[:, 8 + k % 8:9 + k % 8]
        P.ts(rs, ss, 1.0 / D, 1e-6, ALU.mult, ALU.add)
        P.act(rs, rs, AF.Sqrt)
        P.op('dve', lambda e: e.reciprocal(rs, rs), outs=[rs], ins=[rs])
        P.ts(tmp_xn, xt_tile, rs, None, ALU.mult)
        for q4 in range(4):
            b = bank((k * 4 + q4) % 2)
            for j in range(4):
                fc = q4 * 4 + j
                P.transpose(b[:, j * 128:(j + 1) * 128], tmp_xn[:, fc * 128:(fc + 1) * 128], ident)
            for j in range(4):
                fc = q4 * 4 + j
                if (k * 4 + q4) % 2 == 0:
                    P.act(dstT[:, fc, tcol:tcol + 128], b[:, j * 128:(j + 1) * 128], AF.Identity,
                          scale=scol[:, fc:fc + 1], bias=shcol[:, fc:fc + 1])
                else:
                    P.ts(dstT[:, fc, tcol:tcol + 128], b[:, j * 128:(j + 1) * 128],
                         scol[:, fc:fc + 1], shcol[:, fc:fc + 1], ALU.mult, ALU.add)

    xts = [A(R3 + 32768 + i * 8192, 8192, F32) for i in range(2)]
    xns = [A(R3 + 49152 + i * 8192, 8192, F32) for i in range(2)]
    junkb = A(R3 + 65536, 4096, BF16)
    for tt in range(NT):
        xt = xts[tt % 2]
        P.dma(xt, x_d[tt * 128:(tt + 1) * 128, :])
        norm_transpose(xt, hT, tt * 128, s1col, sh1col, xns[tt % 2], junkb, tt)
        if tt % 2 == 1 and late_pieces:
            ada_piece(late_pieces.pop(0))
    while late_pieces:
        ada_piece(late_pieces.pop(0))
    P.tt(modcol[:, 48:80], mc_ps[:, 48:80], cols[:, C_BADA + 48:C_BADA + 80], ALU.add)
    P.stt(s2col, modcol[:, 64:80], 1.0, cols[:, C_GPL:C_GPL + 16], ALU.add, ALU.mult)
    P.copy(sh2col, modcol[:, 48:64])
    if dbg and stop == 1:
        P.dma(dbg_d[:, 0:2048].bitcast(BF16), hT[:, 0:2, :].rearrange("p c t -> p (c t)"))
        P.dma(dbg_d[:, 4096:4128], modcol[:, 0:32])
        P.dma(dbg_d[:, 4144:4176], modcol[:, 48:80])


    def proj_fm(wpiece, evac):
        for r in range(4):
            b = bank(2 + (proj_fm.cnt % 4))
            proj_fm.cnt += 1
            for fc in range(16):
                P.matmul(b, wpiece[:, fc, :], hT[:, fc, r * 512:(r + 1) * 512], start=(fc == 0), stop=(fc == 15))
            evac(r, b)
    proj_fm.cnt = 0

    def load_wpiece(dst, col0, ncol=128, dup64=False):
        if dup64:
            src = win_d[:, col0:col0 + 64].rearrange("(c p) n -> p c n", p=128)
            P.dma(dst[:, :, 0:64], src, q='pool')
            P.dma(dst[:, :, 64:128], src, q='pool')
        else:
            P.dma(dst[:, :, 0:ncol], win_d[:, col0:col0 + ncol].rearrange("(c p) n -> p c n", p=128), q='pool')

    conv = {}
    jobs = []
    if stop >= 5:
        for cq in range(4):
            for c4 in range(4):
                idx = cq * 4 + c4
                jobs.append((('wo', idx), wos[idx].rearrange("p (c n) -> p c n", c=4),
                             wout_d[c4 * 512:(c4 + 1) * 512, cq * 512:(cq + 1) * 512].rearrange("(c p) n -> p c n", p=128)))
        for e8 in range(8):
            for f8 in range(8):
                ffc = e8 * 8 + f8
                jobs.append((('w1', ffc), w1s[ffc].rearrange("p (c n) -> p c n", c=16),
                             wf1_d[:, ffc * 128:(ffc + 1) * 128].rearrange("(c p) n -> p c n", p=128)))
            for fo in range(4):
                for f4 in range(2):
                    idx = (e8 * 4 + fo) * 2 + f4
                    r0 = e8 * 1024 + f4 * 512
                    jobs.append((('w2', idx), w2s[idx].rearrange("p (c n) -> p c n", c=4),
                                 wf2_d[r0:r0 + 512, fo * 512:(fo + 1) * 512].rearrange("(c p) n -> p c n", p=128)))
    jobs.reverse()

    def bg_issue(n=3):
        for _ in range(n):
            if not jobs:
                return
            key, dst, src = jobs.pop()
            conv[key] = P.dma(dst, src, q='pool', chain='bg')
    P.bg_hook = bg_issue

    if stop >= 2:
        T = [A(R3 + i * 8192, 8192, F32) for i in range(7)]
        xrp = A(R3 + 57344, 8192 + 16, F32)
        xcb = A(R3 + 65600, 4096, BF16)
        ssum = A(R3 + 69696, 8192, F32)
        wxy = [A(R3 + 77888 + i * 4096, 4096, BF16).rearrange("p (c n) -> p c n", c=16) for i in range(4)]
        wra = A(R3 + 94272, 2048, BF16).rearrange("p (n j) -> p n j", n=8)
        wrx = A(R3 + 96320, 2048, BF16).rearrange("p (n j) -> p n j", n=8)
        P.dma(wra, wra_d.rearrange("n i j -> i n j"), q='pool')
        P.dma(wrx, wrx_d.rearrange("n i j -> i n j"), q='pool')
        lam = cols[:, C_LAM:C_LAM + 8]
        P.act(cA, lam, AF.Exp, scale=-1.0)
        P.act(cA, cA, AF.Ln, bias=1.0)
        P.ts(cA2, cA, -16.0, None, ALU.mult)
        P.ts(cA, cA, -8.0, None, ALU.mult)
        xrp2 = [xrp, A(R3 + 98368, 8192 + 16, F32)]
        for xp_ in xrp2:
            P.memset(xp_[:, 0:4], 0.0)

        def xr_proj(nb):
            wx = wxy[(2 * nb) % 4]
            load_wpiece(wx, XRc + nb * 128)
            dst = xrp2[nb % 2][:, 4:4 + S]
            proj_fm(wx, lambda r, b: P.copy(dst[:, r * 512:(r + 1) * 512], b, eng='act'))
        xr_proj(0)
        for nb in range(8):
            xrp = xrp2[nb % 2]
            wy = wxy[(2 * nb + 1) % 4]
            load_wpiece(wy, YRc + nb * 128)
            if nb + 1 < 8:
                xr_proj(nb + 1)
            xc = T[0]
            cw = lambda k: cols[:, C_CW + nb * 4 + k:C_CW + nb * 4 + k + 1]
            P.ts(xc, xrp[:, 4:4 + S], cw(3), cols[:, C_CB + nb:C_CB + nb + 1], ALU.mult, ALU.add)
            for k in (2, 1, 0):
                P.stt(xc, xrp[:, 1 + k:1 + k + S], cw(k), xc, ALU.mult, ALU.add)
            P.copy(xcb, xc)
            rg, ig = T[1], T[2]
            for (wr, dst, bc) in ((wra, rg, C_BA), (wrx, ig, C_BX)):
                for r in range(4):
                    b = bank(2 + (proj_fm.cnt % 4))
                    proj_fm.cnt += 1
                    P.matmul(b, wr[:, nb, :], xcb[:, r * 512:(r + 1) * 512])
                    P.act(dst[:, r * 512:(r + 1) * 512], b, AF.Sigmoid, bias=cols[:, bc + nb:bc + nb + 1])
            a_t, a2 = T[3], T[4]
            P.act(a_t, rg, AF.Exp, scale=cA[:, nb:nb + 1])
            P.act(a2, rg, AF.Exp, scale=cA2[:, nb:nb + 1])
            P.ts(a2, a2, -1.0, 1.0, ALU.mult, ALU.add)
            P.act(a2, a2, AF.Sqrt)
            P.tt(ig, ig, xc, ALU.mult)
            P.tt(ig, ig, a2, ALU.mult)
            hh = T[5]
            P.op('dve', lambda e, hh=hh, a_t=a_t, ig=ig: e.tensor_tensor_scan(hh, a_t, ig, 0.0, ALU.mult, ALU.add),
                 outs=[hh], ins=[a_t, ig])
            gy = T[6]
            proj_fm(wy, lambda r, b: P.act(gy[:, r * 512:(r + 1) * 512], b, AF.Gelu_apprx_tanh))
            P.tt(gy, gy, hh, ALU.mult)
            P.ts(orT[:, nb, :], gy, cols[:, C_GRNN + nb:C_GRNN + nb + 1], None, ALU.mult)
            sq = T[1]
            P.act(sq, gy, AF.Square)
            for r in range(4):
                b = bank(2 + (proj_fm.cnt % 4))
                proj_fm.cnt += 1
                P.matmul(b, onesf, sq[:, r * 512:(r + 1) * 512])
                if nb == 0:
                    P.copy(ssum[:, r * 512:(r + 1) * 512], b)
                else:
                    P.tt(ssum[:, r * 512:(r + 1) * 512], ssum[:, r * 512:(r + 1) * 512], b, ALU.add)
        P.ts(ssum, ssum, 1.0 / 1024, 1e-6, ALU.mult, ALU.add)
        P.act(ssum, ssum, AF.Sqrt)
        P.op('dve', lambda e: e.reciprocal(ssum, ssum), outs=[ssum], ins=[ssum])
        for nb in range(8):
            P.tt(orT[:, nb, :], orT[:, nb, :], ssum, ALU.mult)
        if dbg and stop == 2:
            P.dma(dbg_d[:, 0:2048].bitcast(BF16), orT[:, 0:2, :].rearrange("p c t -> p (c t)"))

    if stop >= 3:
        QT = A(R3, 32768, BF16).rearrange("p (c t) -> p c t", c=8)
        KcT = A(R3, 16384, BF16).rearrange("p (g t) -> p g t", g=4)
        VcT = A(R3 + 16384, 16384, BF16).rearrange("p (g t) -> p g t", g=4)
        KsT = A(R3 + 32768, 16384, BF16).rearrange("p (g t) -> p g t", g=4)
        KwT = A(R3 + 49152, 16384, BF16).rearrange("p (g t) -> p g t", g=4)
        Vs = A(R3 + 65536, 8320, BF16).rearrange("p (t g d) -> p t g d", t=16, g=4)
        Vw = A(R3 + 73984, 8320, BF16).rearrange("p (t g d) -> p t g d", t=16, g=4)
        G = A(R3 + 82432, 3072, F32).rearrange("p (t h) -> p t h", t=16)
        KcmpT = A(R3 + 85504, 1024, BF16).rearrange("p (g n) -> p g n", g=4)
        Vcmp = A(R3 + 86528, 800, BF16).rearrange("p (g d) -> p g d", g=4)
        wr3 = [A(R3 + 87360 + i * 4096, 4096, BF16).rearrange("p (c n) -> p c n", c=16) for i in range(3)]
        VT = A(R3 + 99648, 4096, BF16)
        w1k = A(R3 + 32768, 8192, BF16).rearrange("p (l j) -> p l j", l=16)
        w1v = A(R3 + 40960, 8192, BF16).rearrange("p (l j) -> p l j", l=16)
        w2kd = A(R3 + 49152, 512, BF16).rearrange("p (c n) -> p c n", c=2)
        w2vv = A(R3 + 49664, 256, BF16).rearrange("p (c n) -> p c n", c=2)
        pekb = A(R3 + 49920, 32, BF16)
        pevb = A(R3 + 49952, 32, BF16)
        hidT = A(R3 + 50176, 512, BF16).rearrange("p (c n) -> p c n", c=2)
        P.dma(w1k, w1k_d.rearrange("(l p) j -> p l j", p=128), q='pool')
        P.dma(w1v, w1v_d.rearrange("(l p) j -> p l j", p=128), q='pool')
        P.dma(w2kd[:, :, 0:64], w2k_d.rearrange("(c p) n -> p c n", p=128), q='pool')
        P.dma(w2kd[:, :, 64:128], w2k_d.rearrange("(c p) n -> p c n", p=128), q='pool')
        P.dma(w2vv, w2v_d.rearrange("(c p) n -> p c n", p=128), q='pool')
        P.copy(pekb, cols[:, C_PEK:C_PEK + 16])
        P.copy(pevb, cols[:, C_PEV:C_PEV + 16])
        wi = [0]

        def nextw():
            w = wr3[wi[0] % 3]
            wi[0] += 1
            return w

        def evac_dup2(dst, g):
            def f(r, b):
                P.copy(dst[0:64, g, r * 512:(r + 1) * 512], b[0:64, :], eng='act')
                if r == 0:
                    P.copy(dst[64:128, g, 0:511], b[64:128, 1:512])
                else:
                    P.copy(dst[64:128, g, r * 512 - 1:r * 512 + 511], b[64:128, :])
            return f
        for (dst, c0) in ((KcT, KCc), (VcT, VCc)):
            for g in range(4):
                w = nextw()
                load_wpiece(w, c0 + g * 64, dup64=True)
                proj_fm(w, evac_dup2(dst, g))
        for (w1, peb, pb) in ((w1k, pekb, pbk), (w1v, pevb, pbv)):
            b = bank(7, 96)
            for jc in range(2):
                for lp in range(16):
                    P.matmul(b[:, jc:jc + 1], w1[:, lp, jc * 128:(jc + 1) * 128], peb[:, lp:lp + 1],
                             start=(jc == 0 and lp == 0), stop=True, skip_group_check=True)
            P.copy(pb, b[:, 0:2])
        P.memset(Vcmp, 0.0)
        P.memset(KcmpT, 0.0)
        P.memset(Vcmp[:, :, 64:65], 1.0)
        for g in range(4):
            P.dma(Vcmp[:, g, 65:97], kc_d[:, K_W:K_W + 32], q='pool')
        for (src, w1, pb, isk) in ((KcT, w1k, pbk, True), (VcT, w1v, pbv, False)):
            for g in range(4):
                for jc in range(2):
                    b = bank(2 + (proj_fm.cnt % 4))
                    proj_fm.cnt += 1
                    for lp in range(16):
                        P.matmul(b[:, 0:127], w1[:, lp, jc * 128:(jc + 1) * 128],
                                 src[:, g, 2 * lp:2 * lp + 16 * 126 + 1:16], start=(lp == 0), stop=(lp == 15))
                    P.act(hidT[:, jc, 0:127], b[:, 0:127], AF.Gelu_apprx_tanh, bias=pb[:, jc:jc + 1])
                b = bank(2 + (proj_fm.cnt % 4))
                proj_fm.cnt += 1
                if isk:
                    for jc in range(2):
                        P.matmul(b[:, 0:127], w2kd[:, jc, :], hidT[:, jc, 0:127], start=(jc == 0), stop=(jc == 1))
                    P.copy(KcmpT[:, g, 0:127], b[:, 0:127])
                else:
                    for jc in range(2):
                        P.matmul(b[0:127, 0:64], hidT[:, jc, 0:127], w2vv[:, jc, :], start=(jc == 0), stop=(jc == 1))
                    P.copy(Vcmp[0:127, g, 0:64], b[0:127, 0:64])
        for (dst, c0) in ((KsT, KSc), (KwT, KWc)):
            for g in range(4):
                w = nextw()
                load_wpiece(w, c0 + g * 64, dup64=True)

                def f(r, b, dst=dst, g=g):
                    P.copy(dst[:, g, r * 512:(r + 1) * 512], b, eng=('act' if r % 2 else 'dve'))
                proj_fm(w, f)
        P.memset(Vs[:, :, :, 64:65], 1.0)
        P.memset(Vw[:, :, :, 64:65], 1.0)
        for (dst, c0) in ((Vs, VSc), (Vw, VWc)):
            for ch in range(2):
                w = nextw()
                load_wpiece(w, c0 + ch * 128)
                proj_fm(w, lambda r, b: P.copy(VT[:, r * 512:(r + 1) * 512], b, eng='act'))
                for tt in range(NT):
                    b = bank(tt % 2, 128, BF16)
                    P.transpose(b, VT[:, tt * 128:(tt + 1) * 128], identb)
                    P.copy(dst[:, tt, 2 * ch:2 * ch + 2, 0:64], b.rearrange("p (g d) -> p g d", g=2))
        w = nextw()
        load_wpiece(w, GLc, ncol=48)
        GT = A(R3, 8192, F32)
        for r in range(4):
            b = bank(2 + (proj_fm.cnt % 4))
            proj_fm.cnt += 1
            for fc in range(16):
                P.matmul(b[0:48, :], w[:, fc, 0:48], hT[:, fc, r * 512:(r + 1) * 512], start=(fc == 0), stop=(fc == 15))
            P.act(GT[0:48, r * 512:(r + 1) * 512], b[0:48, :], AF.Sigmoid)
        for tt in range(NT):
            b = bank(tt % 2, 48)
            P.transpose(b, GT[0:48, tt * 128:(tt + 1) * 128], ident[0:48, 0:48])
            P.copy(G[:, tt, :], b)
        for c in range(8):
            w = nextw()
            load_wpiece(w, QC + c * 128)

            def f(r, b, c=c):
                if r % 2:
                    P.act(QT[:, c, r * 512:(r + 1) * 512], b, AF.Identity, scale=0.125)
                else:
                    P.ts(QT[:, c, r * 512:(r + 1) * 512], b, 0.125, None, ALU.mult)
            proj_fm(w, f)
        if dbg and stop == 3:
            P.dma(dbg_d[:, 0:1024].bitcast(BF16), QT[:, 0, :])
            P.dma(dbg_d[:, 1024:2048].bitcast(BF16), KsT[:, 0, :])
            P.dma(dbg_d[:, 2048:2048 + 4160 // 2].bitcast(BF16), Vs.rearrange("p t g d -> p (t g d)"))
            P.dma(dbg_d[:, 4200:4200 + 768], G.rearrange("p t h -> p (t h)"))
            P.dma(dbg_d[:, 5000:5256].bitcast(BF16), KcmpT.rearrange("p g n -> p (g n)"))
            P.dma(dbg_d[:, 5300:5500].bitcast(BF16), Vcmp.rearrange("p g d -> p (g d)"))

    if stop >= 4:
        oaT = A(R1, 32768, BF16).rearrange("p (c t) -> p c t", c=8)
        B0 = R1 + 32768
        maskc = A(B0, 4096, BF16)
        Eexp = A(B0 + 4096, 4096, BF16).rearrange("p (k n) -> p k n", k=16)
        Am = A(B0 + 8192, 2048, F32).rearrange("p (t j) -> p t j", t=16)
        Bm = A(B0 + 10240, 2048, F32).rearrange("p (t j) -> p t j", t=16)
        caus4 = A(B0 + 12288, 4096, BF16).rearrange("p (o q) -> p o q", o=4)
        wmask = A(B0 + 16384, 8192, BF16).rearrange("p (o q) -> p o q", o=8)
        negmT = A(B0 + 24576, 4096, BF16).rearrange("p (g q) -> p g q", g=4)
        otmp = A(B0 + 28672, 1024, F32)
        sm = A(B0 + 29696, 1024, F32)
        oacc = [A(R3 + 87360 + i * 4096, 4096, F32) for i in range(4)]
        PT = [A(R3 + 103744 + i * 1024, 1024, BF16) for i in range(5)]
        onrm = A(R3 + 103744, 4096, F32)
        P.dma(maskc, kc_d[:, K_MASKC:K_MASKC + 2048], q='pool')
        P.dma(Eexp.rearrange("p k n -> p (k n)"), kc_d[:, K_E:K_E + 2048], q='pool')
        P.dma(Am.rearrange("p t j -> p (t j)"), kc_d[:, K_A:K_A + 512])
        P.dma(Bm.rearrange("p t j -> p (t j)"), kc_d[:, K_B:K_B + 512])
        P.dma(caus4.rearrange("p o q -> p (o q)"), kc_d[:, K_CAUS4:K_CAUS4 + 2048], q='pool')
        P.dma(wmask.rearrange("p o q -> p (o q)"), kc_d[:, K_WM:K_WM + 4096], q='pool')
        pt_i = [0]
        P.memset(negmT, 0.0)
        PVB = [bank(4 + j) for j in range(4)]
        PT = PT + [A(B0 + 30720 + 2048 - 1024, 1024, BF16)]
        NPT = len(PT)

        def score_one(KT, kcols, masks, QB, g, h, jv):
            q0, q1 = jv[0] * 128, (jv[-1] + 1) * 128
            bk = bank(h)
            first = True
            for (ml, mr) in masks:
                P.matmul(bk[:, q0:q1], ml, mr[:, q0:q1], start=first, stop=True, skip_group_check=True)
                first = False
            hh = 4 * g + h
            c, base = hh // 2, 64 * (hh % 2)
            P.matmul(bk[:, q0:q1], KT[base:base + 64, g, kcols], QT[base:base + 64, c, QB * 512 + q0:QB * 512 + q1],
                     start=first, stop=True, skip_group_check=True)
            pt = PT[pt_i[0] % NPT]
            pt_i[0] += 1
            P.act(pt[:, q0:q1], bk[:, q0:q1], AF.Exp)
            return pt

        def run_branch(iters, QB, g, width):
            started = [False] * 4
            pending = [None]

            def do_pv():
                if pending[0] is None:
                    return
                pts, hs, V, jv = pending[0]
                for j in jv:
                    pv = PVB[j][:, 0:4 * width].rearrange("p (h d) -> p h d", h=4)
                    for pt, h in zip(pts, hs):
                        P.matmul(pv[:, h, :], pt[:, j * 128:(j + 1) * 128], V,
                                 start=(not started[j]), stop=True, skip_group_check=True)
                        started[j] = True
                pending[0] = None
            for (KT, kcols, masks, V, jv) in iters:
                for hs in ((0, 1), (2, 3)):
                    pts = [score_one(KT, kcols, masks, QB, g, h, jv) for h in hs]
                    do_pv()
                    pending[0] = (pts, hs, V, jv)
            do_pv()

        def finish_branch(pv, qt, g, br, oa, first, den, wg):
            P.ts(den, pv[:, :, 64], 1e-30, None, ALU.max)
            P.op('dve', lambda e: e.reciprocal(den, den), outs=[den], ins=[den])
            P.tt(wg, den, G[:, qt, 12 * g + br:12 * g + br + 10:3], ALU.mult)
            dst = oa.rearrange("p (h d) -> p h d", h=16)[:, 4 * g:4 * g + 4, :]
            wgb = wg.unsqueeze(2).broadcast_to([128, 4, 64])
            if first:
                P.tt(dst, pv[:, :, 0:64], wgb, ALU.mult)
            else:
                t3 = otmp.rearrange("p (h d) -> p h d", h=4)
                P.tt(t3, pv[:, :, 0:64], wgb, ALU.mult)
                P.tt(dst, dst, t3, ALU.add)

        den_c = lambda j: sm[:, 4 * j:4 * j + 4]

        def cmp_part(QB, g):
            run_branch([(KcmpT, slice(0, 128), [(identb, maskc[:, QB * 512:(QB + 1) * 512])],
                         Vcmp[:, g, 0:97], list(range(4)))], QB, g, 97)

        def cmp_finish(QB, g):
            for j in range(4):
                qt = 4 * QB + j
                pvc = PVB[j][:, 0:388].rearrange("p (h d) -> p h d", h=4)
                finish_branch(pvc, qt, g, 0, oacc[j], True, den_c(j), sm[:, 16:20])
                imp = sm[:, 64 + 32 * j:96 + 32 * j]
                P.ts(imp, pvc[:, 0, 65:97], den_c(j)[:, 0:1], None, ALU.mult)
                for h in range(1, 4):
                    P.stt(imp, pvc[:, h, 65:97], den_c(j)[:, h:h + 1], imp, ALU.mult, ALU.add)
            for j in range(4):
                qt = 4 * QB + j
                imp = sm[:, 64 + 32 * j:96 + 32 * j]
                P.tt(imp, imp, Am[:, qt, :], ALU.mult)
                P.tt(imp, imp, Bm[:, qt, :], ALU.add)
                top8 = sm[:, 24 + 8 * j:32 + 8 * j]
                P.op('dve', lambda e, top8=top8, imp=imp: e.max(top8, imp), outs=[top8], ins=[imp])
                P.ts(imp, imp, top8[:, 7:8], None, ALU.is_ge)
                P.ts(imp, imp, -NEGV, NEGV, ALU.mult, ALU.add)

        def sel_masks_T(QB, g):
            for j in range(4):
                negm = sm[:, 64 + 32 * j:96 + 32 * j]
                bt = bank(j)[:, 0:128]
                P.transpose(bt[0:32, :], negm, ident)
                P.copy(negmT[0:32, g, j * 128:(j + 1) * 128], bt[0:32, :], eng='act')

        def win_part(QB, g):
            iters = []
            for o in range(8):
                kt = 4 * QB - 4 + o
                if kt < 0:
                    continue
                jv = [j for j in range(4) if 0 <= j + 4 - o <= 4]
                iters.append((KwT, slice(kt * 128, (kt + 1) * 128), [(identb, wmask[:, o, :])], Vw[:, kt, g, :], jv))
            run_branch(iters, QB, g, 65)
            for j in range(4):
                pvw = PVB[j][:, 0:260].rearrange("p (h d) -> p h d", h=4)
                finish_branch(pvw, 4 * QB + j, g, 2, oacc[j], False, sm[:, 208:212], sm[:, 212:216])

        def sel_part(QB, g):
            iters = []
            for kt in range(4 * QB + 4):
                masks = [(Eexp[:, kt, :], negmT[:, g, :])]
                if kt >= 4 * QB:
                    masks.append((identb, caus4[:, kt - 4 * QB, :]))
                jv = [j for j in range(4) if 4 * QB + j >= kt]
                iters.append((KsT, slice(kt * 128, (kt + 1) * 128), masks, Vs[:, kt, g, :], jv))
            run_branch(iters, QB, g, 65)
            for j in range(4):
                pvs = PVB[j][:, 0:260].rearrange("p (h d) -> p h d", h=4)
                finish_branch(pvs, 4 * QB + j, g, 1, oacc[j], False, sm[:, 216:220], sm[:, 220:224])

        for QB in range(4):
            for g in range(4):
                cmp_part(QB, g)
                cmp_finish(QB, g)
                win_part(QB, g)
                sel_masks_T(QB, g)
                sel_part(QB, g)
            for j in range(4):
                qt = 4 * QB + j
                oa = oacc[j]
                ss = sm[:, 228 + j:229 + j]
                P.act(onrm, oa, AF.Square, accum_out=ss)
                P.ts(ss, ss, 1.0 / 1024, 1e-6, ALU.mult, ALU.add)
                P.act(ss, ss, AF.Sqrt)
                P.op('dve', lambda e, ss=ss: e.reciprocal(ss, ss), outs=[ss], ins=[ss])
                P.ts(onrm, oa, ss, None, ALU.mult)
                for half in range(2):
                    b = bank(half)
                    for jj in range(4):
                        c = half * 4 + jj
                        P.transpose(b[:, jj * 128:(jj + 1) * 128], onrm[:, c * 128:(c + 1) * 128], ident)
                    for jj in range(4):
                        c = half * 4 + jj
                        P.ts(oaT[:, c, qt * 128:(qt + 1) * 128], b[:, jj * 128:(jj + 1) * 128],
                             cols[:, C_GATT + c:C_GATT + c + 1], None, ALU.mult)
        if dbg and stop == 4:
            P.dma(dbg_d[:, 0:4096].bitcast(BF16), oaT[:, 0:4, :].rearrange("p c t -> p (c t)"))

    if stop >= 5:
        E0 = R1 + 32768
        GM1 = A(E0, 8192, F32)
        GM2 = A(E0 + 8192, 8192, F32)
        P.bg_hook = None
        bg_issue(10 ** 6)
        r42 = [A(E0 + 16384 + i * 4096, 4096, BF16).rearrange("p (c n) -> p c n", c=4) for i in range(4)]
        x1 = [A(R3 + i * 8192, 8192, F32) for i in range(4)]
        fb = [A(R3 + 32768 + i * 8192, 8192, F32) for i in range(4)]
        h2T = A(R3 + 65536, 16384, BF16).rearrange("p (c t) -> p c t", c=16)
        uT = A(R3 + 81920, 8192, BF16).rearrange("p (c t) -> p c t", c=8)
        w1_r = [A(R3 + 90112 + i * 4096, 4096, BF16).rearrange("p (c n) -> p c n", c=16) for i in range(4)]
        rtmp = A(R3 + 106496, 2048, F32)
        junk2 = A(R3 + 81920, 4096, BF16)
        crep = A(R3 + 104448, 4096, BF16).rearrange("p (c m) -> p c m", c=16)
        P.copy(crep, cact.unsqueeze(2).broadcast_to([128, 16, 128]))
        wa2 = [A(R3 + i * 16384, 16384, BF16).rearrange("p (c n) -> p c n", c=16) for i in range(2)]
        brow = A(R3 + 32768, 2048, F32)
        grow = A(R3 + 34816, 2048, F32)
        for k, pj in enumerate([8, 9, 10, 11, 20, 21, 22, 23]):
            w = wa2[k % 2]
            P.dma(w, wada_d[:, pj * 512:(pj + 1) * 512].rearrange("(c p) n -> p c n", p=128), q='pool')
            P.dma(brow, bgt_d[:, k * 512:(k + 1) * 512])
            P.dma(grow, gpost_d[:, k * 512:(k + 1) * 512])
            b = bank(k % 2)
            for fc in range(16):
                P.matmul(b, crep[:, fc, :], w[:, fc, :], start=(fc == 0), stop=(fc == 15))
            dst = (GM1 if k < 4 else GM2)[:, (k % 4) * 512:(k % 4 + 1) * 512]
            P.tt(dst, b, brow, ALU.add)
            P.tt(dst, dst, grow, ALU.mult)
        oT = lambda c: (oaT[:, c, :] if c < 8 else orT[:, c - 8, :])
        wo_i, w1_i, w2_i, fi = [0], [0], [0], [0]
        for tb in range(4):
            t0 = tb * 512
            for tt in range(4):
                P.dma(x1[tt], x_d[t0 + tt * 128:t0 + (tt + 1) * 128, :])
            for cq in range(4):
                pb = [bank(4 + tt) for tt in range(4)]
                for c4 in range(4):
                    w = r42[wo_i[0] % 4]
                    wo_i[0] += 1
                    P.dma(w, wos[cq * 4 + c4].rearrange("p (c n) -> p c n", c=4), deps=[conv[('wo', cq * 4 + c4)]])
                    for cc in range(4):
                        c = c4 * 4 + cc
                        for tt in range(4):
                            P.matmul(pb[tt], oT(c)[:, t0 + tt * 128:t0 + (tt + 1) * 128], w[:, cc, :],
                                     start=(c == 0), stop=(c == 15))
                for tt in range(4):
                    P.copy(fb[tt][:, cq * 512:(cq + 1) * 512], pb[tt], eng=('act' if tt % 2 else 'dve'))
            for tt in range(4):
                ss = stat[:, 16 + tt:17 + tt]
                P.act(junk2, fb[tt], AF.Square, accum_out=ss)
                P.ts(ss, ss, 1.0 / D, 1e-6, ALU.mult, ALU.add)
                P.act(ss, ss, AF.Sqrt)
                P.op('dve', lambda e, ss=ss: e.reciprocal(ss, ss), outs=[ss], ins=[ss])
                P.stt(fb[tt], fb[tt], ss, GM1, ALU.mult, ALU.mult)
                P.tt(x1[tt], x1[tt], fb[tt], ALU.add)
                norm_transpose(x1[tt], h2T, tt * 128, s2col, sh2col, fb[tt], junk2, tt)
            for e8 in range(8):
                for f8 in range(8):
                    ffc = e8 * 8 + f8
                    w = w1_r[w1_i[0] % 4]
                    w1_i[0] += 1
                    P.dma(w, w1s[ffc].rearrange("p (c n) -> p c n", c=16), deps=[conv[('w1', ffc)]])
                    b = bank(fi[0] % 2)
                    fi[0] += 1
                    for fc in range(16):
                        P.matmul(b, w[:, fc, :], h2T[:, fc, :], start=(fc == 0), stop=(fc == 15))
                    P.act(rtmp, b, AF.Relu)
                    P.tt(uT[:, f8, :], rtmp, rtmp, ALU.mult)
                for fo in range(4):
                    pb = [bank(4 + tt) for tt in range(4)]
                    for f4 in range(2):
                        w = r42[wo_i[0] % 4]
                        wo_i[0] += 1
                        P.dma(w, w2s[(e8 * 4 + fo) * 2 + f4].rearrange("p (c n) -> p c n", c=4),
                              deps=[conv[('w2', (e8 * 4 + fo) * 2 + f4)]])
                        for cc in range(4):
                            f8 = f4 * 4 + cc
                            for tt in range(4):
                                P.matmul(pb[tt], uT[:, f8, tt * 128:(tt + 1) * 128], w[:, cc, :],
                                         start=(f8 == 0), stop=(f8 == 7))
                    for tt in range(4):
                        dst = fb[tt][:, fo * 512:(fo + 1) * 512]
                        if e8 == 0:
                            P.copy(dst, pb[tt], eng=('act' if tt % 2 else 'dve'))
                        else:
                            P.tt(dst, dst, pb[tt], ALU.add)
            for tt in range(4):
                ss = stat[:, 24 + tt:25 + tt]
                P.act(junk2, fb[tt], AF.Square, accum_out=ss)
                P.ts(ss, ss, 1.0 / D, 1e-6, ALU.mult, ALU.add)
                P.act(ss, ss, AF.Sqrt)
                P.op('dve', lambda e, ss=ss: e.reciprocal(ss, ss), outs=[ss], ins=[ss])
                P.stt(fb[tt], fb[tt], ss, GM2, ALU.mult, ALU.mult)
                P.tt(fb[tt], fb[tt], x1[tt], ALU.add)
                P.dma(out_d[t0 + tt * 128:t0 + (tt + 1) * 128, :], fb[tt], is_output=True)
    if stop < 5:
        P.dma(out_d[0:128, :], A(R3, 8192, F32), is_output=True)
    if dbg:
        P.out_dmas = [o for o in P.ops['sp'] if o.is_dma] + [o for o in P.out_dmas]
    P.emit()
    st.close()
    return nc, P


def host_consts():
    ident = np.eye(128, dtype=np.float32)
    kc = np.zeros((128, NKC), np.float32)
    key = np.arange(128)[:, None]
    q = np.arange(128)[None, :]
    cm = np.where(key > q, NEGV, 0.0)
    wm = np.where(key <= q, NEGV, 0.0)
    full = np.full((128, 128), NEGV)
    zero = np.zeros((128, 128))
    for o in range(4):
        kc[:, K_CAUS4 + o * 512:K_CAUS4 + (o + 1) * 512] = np.concatenate(
            [full if j < o else (cm if j == o else zero) for j in range(4)], 1)
    for o in range(8):
        tiles = []
        for j in range(4):
            dd = j + 4 - o
            tiles.append(full if (dd < 0 or dd > 4) else (cm if dd == 0 else (wm if dd == 4 else zero)))
        kc[:, K_WM + o * 512:K_WM + (o + 1) * 512] = np.concatenate(tiles, 1)
    n = np.arange(128)[:, None]
    pos = np.arange(2048)[None, :]
    kc[:, K_MASKC:K_MASKC + 2048] = np.where(16 * n + 31 <= pos, 0.0, NEGV)
    posq = np.arange(2048)
    blk = np.arange(32)[None, :]
    cur = (posq // 64)[:, None]
    forced = (blk == 0) | (blk == cur) | (blk == cur - 1)
    valid = blk * 64 <= posq[:, None]
    Am = np.where(forced | ~valid, 0.0, 1.0).astype(np.float32)
    Bm = np.where(forced, 1e9, np.where(valid, 0.0, -1e9)).astype(np.float32)
    kc[:, K_A:K_A + 512] = Am.reshape(16, 128, 32).transpose(1, 0, 2).reshape(128, 512)
    kc[:, K_B:K_B + 512] = Bm.reshape(16, 128, 32).transpose(1, 0, 2).reshape(128, 512)
    E = np.zeros((128, 16, 128), np.float32)
    for kt in range(16):
        for k in range(128):
            E[(kt * 128 + k) // 64, kt, k] = 1.0
    kc[:, K_E:K_E + 2048] = E.reshape(128, 2048)
    c0 = np.arange(127)[:, None] * 16
    s0 = np.arange(32)[None, :] * 64
    ov = np.minimum(c0 + 32, s0 + 64) - np.maximum(c0, s0)
    kc[0:127, K_W:K_W + 32] = np.clip(ov, 0, None).astype(np.float32) / 32
    return ident, kc


def make_in_maps(inp):
    f = lambda a: np.ascontiguousarray(np.asarray(a, dtype=np.float32))
    ident, kc = host_consts()
    col = lambda v: f(v).reshape(-1, 128).T
    b_ada = f(inp['b_ada'])[0]
    shared_cols = np.zeros((128, NCOLS), np.float32)
    shared_cols[:, C_BADA:C_BADA + 96] = col(b_ada)
    shared_cols[:, C_GPM:C_GPM + 16] = col(inp['g_pre_mix'][0])
    shared_cols[:, C_GPL:C_GPL + 16] = col(inp['g_pre_mlp'][0])
    cw = f(inp['conv_w'])[0]
    shared_cols[:, C_CW:C_CW + 32] = cw.T.reshape(8, 128, 4).transpose(1, 0, 2).reshape(128, 32)
    shared_cols[:, C_CB:C_CB + 8] = col(inp['conv_b'][0])
    shared_cols[:, C_BA:C_BA + 8] = col(inp['b_rg_a'][0])
    shared_cols[:, C_BX:C_BX + 8] = col(inp['b_rg_x'][0])
    shared_cols[:, C_LAM:C_LAM + 8] = col(inp['lru_lambda'][0])
    shared_cols[:, C_GRNN:C_GRNN + 8] = col(inp['g_grp_rnn'][0])
    shared_cols[:, C_GATT:C_GATT + 8] = col(inp['g_grp_att'][0])
    shared_cols[:, C_PEK:C_PEK + 16] = col(f(inp['cmp_pe_k'])[0].reshape(-1))
    shared_cols[:, C_PEV:C_PEV + 16] = col(f(inp['cmp_pe_v'])[0].reshape(-1))
    bgt = np.concatenate([b_ada[4096:6144], b_ada[10240:12288]])
    bgt_rows = np.ascontiguousarray(np.broadcast_to(bgt[None, :], (128, 4096)))
    gpost = np.concatenate([f(inp['g_post_mix'])[0], f(inp['g_post_mlp'])[0]])
    gpost_rows = np.ascontiguousarray(np.broadcast_to(gpost[None, :], (128, 4096)))
    shared = {
        "kconst": kc, "ident": ident, "w_ada": f(inp['w_ada'])[0], "bgt_rows": bgt_rows, "gpost_rows": gpost_rows,
        "w_in": f(inp['w_in'])[0], "cmp_w1_k": f(inp['cmp_w1_k'])[0], "cmp_w2_k": f(inp['cmp_w2_k'])[0],
        "cmp_w1_v": f(inp['cmp_w1_v'])[0], "cmp_w2_v": f(inp['cmp_w2_v'])[0],
        "w_rg_a": f(inp['w_rg_a'])[0], "w_rg_x": f(inp['w_rg_x'])[0], "w_out": f(inp['w_out'])[0],
        "w_ff1": f(inp['w_ff1'])[0], "w_ff2": f(inp['w_ff2'])[0],
    }
    x = f(inp['x'])
    c = f(inp['c'])
    maps = []
    for b in range(8):
        cols_b = shared_cols.copy()
        cols_b[:, C_C:C_C + 16] = col(c[b])
        m = dict(shared)
        m["x"] = x[b]
        m["cols"] = cols_b
        maps.append(m)
    return maps


def kernel(**inputs):
    nc = bass.Bass("TRN2", target_bir_lowering=False)
    build(nc)
    maps = make_in_maps(inputs)
    res = run_bass_kernel_spmd(nc, maps, core_ids=list(range(8)))
    return np.stack([np.asarray(r["out"], dtype=np.float32) for r in res.results], axis=0)
```

```python
import numpy as np
import concourse.bass as bass
import concourse.mybir as mybir
from concourse.bass_utils import run_bass_kernel_spmd

F32 = mybir.dt.float32
BF16 = mybir.dt.bfloat16
U8 = mybir.dt.uint8
AF = mybir.ActivationFunctionType
ALU = mybir.AluOpType
AX = mybir.AxisListType
ESZ = {F32: 4, BF16: 2, U8: 1, mybir.dt.int32: 4, mybir.dt.uint32: 4}
PAGE = 2048


def region(ap):
    t = ap.tensor
    sp = str(ap.space)
    if 'DRAM' in sp.upper() or 'HBM' in sp.upper() or type(t).__name__.startswith('DRam'):
        return None
    esz = ESZ[ap.dtype]
    rowlen = 1
    for s in list(t.shape)[1:]:
        rowlen *= int(s)
    pairs = [(int(a), int(b)) for a, b in ap.ap]
    off = int(ap.offset)
    p_lo = off // rowlen
    col = off % rowlen
    pstep, pcnt = pairs[0]
    if pstep == 0:
        pcnt = 1
    ext = 1
    for st, cnt in pairs[1:]:
        ext += (cnt - 1) * abs(st)
    lo, hi = col * esz, (col + ext) * esz
    if 'PSum' in type(t).__name__:
        return (t.name, 0, 128, lo // 2048 * 2048, ((hi - 1) // 2048 + 1) * 2048)
    return (t.name, p_lo, p_lo + pcnt, lo, hi)


def overlap(a, b):
    return a[0] == b[0] and a[1] < b[2] and b[1] < a[2] and a[3] < b[4] and b[3] < a[4]


def contains(outer, inner):
    return (outer[0] == inner[0] and outer[1] <= inner[1] and inner[2] <= outer[2]
            and outer[3] <= inner[3] and inner[4] <= outer[4])


class Op:
    __slots__ = ('eng', 'fn', 'deps', 'signal', 'need', 'is_dma', 'idx', 'prev_dma', 'chain')

    def __init__(self, eng, fn, is_dma=False):
        self.eng = eng
        self.fn = fn
        self.deps = set()
        self.signal = None
        self.need = False
        self.is_dma = is_dma
        self.prev_dma = None
        self.chain = eng


class Prog:
    ENGS = ('pe', 'act', 'dve', 'pool', 'sp')
    NDSEM = 8

    def __init__(self, nc):
        self.nc = nc
        self.ops = {e: [] for e in self.ENGS}
        self.wrecs = {}
        self.rrecs = {}
        self.all_ops = []
        self.out_dmas = []
        self.bg_hook = None

    def _pages(self, r):
        return range(r[3] // PAGE, (r[4] - 1) // PAGE + 1)

    def _add_deps(self, op, reads, writes):
        for r in reads:
            for pg in self._pages(r):
                for (reg, o) in self.wrecs.get((r[0], pg), ()):
                    if o is not op and overlap(r, reg):
                        op.deps.add(o)
        for w in writes:
            for pg in self._pages(w):
                key = (w[0], pg)
                for (reg, o) in self.wrecs.get(key, ()):
                    if o is not op and overlap(w, reg):
                        op.deps.add(o)
                for (reg, o) in self.rrecs.get(key, ()):
                    if o is not op and overlap(w, reg):
                        op.deps.add(o)
        for w in writes:
            for pg in self._pages(w):
                key = (w[0], pg)
                if key in self.wrecs:
                    self.wrecs[key] = [x for x in self.wrecs[key] if not contains(w, x[0])]
                if key in self.rrecs:
                    self.rrecs[key] = [x for x in self.rrecs[key] if not contains(w, x[0])]
                self.wrecs.setdefault(key, []).append((w, op))
        for r in reads:
            for pg in self._pages(r):
                key = (r[0], pg)
                lst = self.rrecs.get(key)
                if lst:
                    self.rrecs[key] = [x for x in lst
                                       if not (x[1].eng == op.eng and not x[1].is_dma and not op.is_dma
                                               and contains(r, x[0]))]
                self.rrecs.setdefault(key, []).append((r, op))

    def op(self, eng, fn, outs=(), ins=(), is_dma=False):
        o = Op(eng, fn, is_dma)
        reads = [x for x in (region(a) for a in ins if a is not None) if x is not None]
        writes = [x for x in (region(a) for a in outs if a is not None) if x is not None]
        self._add_deps(o, reads, writes)
        if eng == 'pe':
            o.deps = {d for d in o.deps if not (d.eng == 'pe' and not d.is_dma)}
        self.ops[eng].append(o)
        self.all_ops.append(o)
        return o

    def dma(self, out, in_, q='sp', is_output=False, chain=None, deps=()):
        o = self.op(q, lambda e: e.dma_start(out=out, in_=in_), outs=[out], ins=[in_], is_dma=True)
        if chain is not None:
            o.chain = chain
        o.deps.update(deps)
        if q == 'pool' and chain is None and self.bg_hook is not None:
            self.bg_hook()
        if is_output:
            self.out_dmas.append(o)
        return o

    def matmul(self, out, lhsT, rhs, start=True, stop=True, **kw):
        return self.op('pe', lambda e: e.matmul(out, lhsT, rhs, start=start, stop=stop, **kw),
                       outs=[out], ins=[lhsT, rhs])

    def transpose(self, out, in_, ident):
        return self.op('pe', lambda e: e.transpose(out, in_, ident), outs=[out], ins=[in_, ident])

    def act(self, out, in_, func, bias=None, scale=None, accum_out=None, eng='act'):
        kw = {}
        ins = [in_]
        if bias is not None:
            kw['bias'] = bias
            if not isinstance(bias, (int, float)):
                ins.append(bias)
        if scale is not None:
            kw['scale'] = scale
            if not isinstance(scale, (int, float)):
                ins.append(scale)
        outs = [out]
        if accum_out is not None:
            kw['accum_out'] = accum_out
            outs.append(accum_out)
        return self.op(eng, lambda e: e.activation(out, in_, func, **kw), outs=outs, ins=ins)

    def ts(self, out, in0, s1, s2, op0, op1=None, eng='dve', accum_out=None):
        ins = [in0] + [s for s in (s1, s2) if s is not None and not isinstance(s, (int, float))]
        outs = [out] + ([accum_out] if accum_out is not None else [])
        kw = {}
        if accum_out is not None:
            kw['accum_out'] = accum_out
        if op1 is None:
            return self.op(eng, lambda e: e.tensor_scalar(out, in0, s1, s2, op0, **kw), outs=outs, ins=ins)
        return self.op(eng, lambda e: e.tensor_scalar(out, in0, s1, s2, op0, op1, **kw), outs=outs, ins=ins)

    def tt(self, out, in0, in1, op, eng='dve'):
        return self.op(eng, lambda e: e.tensor_tensor(out, in0, in1, op), outs=[out], ins=[in0, in1])

    def stt(self, out, in0, scalar, in1, op0, op1, eng='dve'):
        ins = [in0, in1] + ([scalar] if not isinstance(scalar, (int, float)) else [])
        return self.op(eng, lambda e: e.scalar_tensor_tensor(out, in0, scalar, in1, op0, op1),
                       outs=[out], ins=ins)

    def copy(self, out, in_, eng='dve'):
        if eng == 'act':
            return self.op('act', lambda e: e.copy(out, in_), outs=[out], ins=[in_])
        return self.op(eng, lambda e: e.tensor_copy(out, in_), outs=[out], ins=[in_])

    def memset(self, out, val, eng='dve'):
        return self.op(eng, lambda e: e.memset(out, val), outs=[out], ins=[])

    def generic(self, eng, fn, outs, ins):
        return self.op(eng, fn, outs=outs, ins=ins)

    def emit(self):
        nc = self.nc
        for e in self.ENGS:
            for i, o in enumerate(self.ops[e]):
                o.idx = i
        for o in self.all_ops:
            best = {}
            keep = set()
            for d in o.deps:
                if d.is_dma:
                    keep.add(d)
                elif d.eng not in best or best[d.eng].idx < d.idx:
                    best[d.eng] = d
            keep.update(best.values())
            o.deps = keep
            for d in keep:
                d.need = True
        fin = Op('sp', None)
        fin.deps = set(self.out_dmas)
        for d in fin.deps:
            d.need = True
        import contextlib
        with contextlib.ExitStack() as st:
            esem = {e: st.enter_context(nc.semaphore('s_' + e)) for e in self.ENGS}
            dsem = {}
            chains = sorted({o.chain for o in self.all_ops if o.is_dma})
            for q in chains:
                dsem[q] = [st.enter_context(nc.semaphore('d_%s%d' % (q, i))) for i in range(self.NDSEM)]
            dcnt = {q: [0] * self.NDSEM for q in chains}
            last = {q: [None] * self.NDSEM for q in chains}
            kk = {q: 0 for q in chains}
            for e in self.ENGS:
                cnt = 0
                for o in self.ops[e]:
                    if o.is_dma:
                        q = o.chain
                        i = kk[q] % self.NDSEM
                        kk[q] += 1
                        dcnt[q][i] += 16
                        o.signal = (dsem[q][i], dcnt[q][i])
                        o.prev_dma = last[q][i]
                        last[q][i] = o
                    elif o.need:
                        cnt += 1
                        o.signal = (esem[e], cnt)
            self.n_waits = 0
            block = st.enter_context(nc.Block())

            def run(e, eng):
                known = {}
                lst = list(self.ops[e])
                if e == 'sp':
                    lst.append(fin)
                for o in lst:
                    need = {}
                    deps = set(o.deps)
                    if o.prev_dma is not None:
                        deps.add(o.prev_dma)
                    for d in deps:
                        s, v = d.signal
                        key = id(s)
                        if key not in need or need[key][1] < v:
                            need[key] = (s, v)
                    for key, (s, v) in need.items():
                        if known.get(key, 0) < v:
                            eng.wait_ge(s, v)
                            self.n_waits += 1
                            known[key] = v
                    if o.fn is None:
                        continue
                    inst = o.fn(eng)
                    if o.is_dma:
                        inst.then_inc(o.signal[0], 16)
                    elif o.need:
                        inst.then_inc(o.signal[0], 1)

            @block.tensor
            def _(eng):
                run('pe', eng)

            @block.scalar
            def _(eng):
                run('act', eng)

            @block.vector
            def _(eng):
                run('dve', eng)

            @block.gpsimd
            def _(eng):
                run('pool', eng)

            @block.sync
            def _(eng):
                run('sp', eng)

S = 2048
D = 2048
NT = 16
QC, KCc, VCc, KSc, VSc, KWc, VWc, GLc, XRc, YRc = 0, 1024, 1280, 1536, 1792, 2048, 2304, 2560, 2608, 3632
NEGV = -30000.0
C_BADA, C_GPM, C_GPL, C_CW, C_CB, C_BA, C_BX, C_LAM, C_GRNN, C_GATT, C_C, C_PEK, C_PEV, NCOLS = \
    0, 96, 112, 128, 160, 168, 176, 184, 192, 200, 208, 224, 240, 256
K_CAUS4, K_WM, K_MASKC, K_A, K_B, K_E, K_W, NKC = 0, 2048, 6144, 8192, 8704, 9216, 11264, 11296


def build(nc, stop=99, dbg=False):
    import contextlib
    dt = lambda name, shape, kind="ExternalInput": nc.dram_tensor(name, shape, F32, kind=kind).ap()
    x_d = dt("x", [S, D])
    cols_d = dt("cols", [128, NCOLS])
    kc_d = dt("kconst", [128, NKC])
    ident_d = dt("ident", [128, 128])
    wada_d = dt("w_ada", [D, 6 * D])
    bgt_d = dt("bgt_rows", [128, 2 * D])
    gpost_d = dt("gpost_rows", [128, 2 * D])
    win_d = dt("w_in", [D, 4656])
    w1k_d = dt("cmp_w1_k", [2048, 256])
    w2k_d = dt("cmp_w2_k", [256, 64])
    w1v_d = dt("cmp_w1_v", [2048, 256])
    w2v_d = dt("cmp_w2_v", [256, 64])
    wra_d = dt("w_rg_a", [8, 128, 128])
    wrx_d = dt("w_rg_x", [8, 128, 128])
    wout_d = dt("w_out", [D, D])
    wf1_d = dt("w_ff1", [D, 4 * D])
    wf2_d = dt("w_ff2", [4 * D, D])
    out_d = dt("out", [S, D], kind="ExternalOutput")
    dbg_d = dt("dbg", [128, 8192], kind="ExternalOutput") if dbg else None
    w1s = nc.dram_tensor("w1s", [64, 128, 2048], BF16).ap()
    w2s = nc.dram_tensor("w2s", [64, 128, 2048], BF16).ap()
    wos = nc.dram_tensor("wos", [16, 128, 2048], BF16).ap()
    gm_s = nc.dram_tensor("gm_s", [2, 128, 2048], F32).ap()

    st = contextlib.ExitStack()
    sb = st.enter_context(nc.sbuf_tensor("sb", [128, 212000], U8))
    ps = st.enter_context(nc.psum_tensor("ps", [128, 4096], F32))
    P = Prog(nc)

    def A(off, nbytes, d):
        assert off + nbytes <= 212000, (off, nbytes)
        return sb[:, off:off + nbytes].bitcast(d)

    def bank(i, n=512, d=F32):
        a = ps[:, i * 512:(i + 1) * 512]
        if d is BF16:
            return a.bitcast(BF16)[:, 0:n]
        return a[:, 0:n]

    ident = A(0, 512, F32)
    identb = A(512, 256, BF16)
    cols = A(768, NCOLS * 4, F32)
    modcol = A(1792, 384, F32)
    s1col = A(2176, 64, F32)
    sh1col = A(2240, 64, F32)
    s2col = A(2304, 64, F32)
    sh2col = A(2368, 64, F32)
    cA = A(2432, 32, F32)
    cA2 = A(2464, 32, F32)
    cact = A(2496, 32, BF16)
    stat = A(2560, 256, F32)
    onesf = A(2816, 512, F32)
    pbk = A(3328, 8, F32)
    pbv = A(3336, 8, F32)
    ccf = A(3344, 64, F32)
    R1 = 4096
    R2 = R1 + 65536
    R3 = R2 + 32768
    hT = A(R1, 65536, BF16).rearrange("p (c t) -> p c t", c=16)
    orT = A(R2, 32768, BF16).rearrange("p (c t) -> p c t", c=8)

    P.dma(ident, ident_d)
    P.dma(cols, cols_d)
    P.copy(identb, ident)
    P.memset(onesf, 1.0)

    P.act(ccf, cols[:, C_C:C_C + 16], AF.Silu)
    P.copy(cact, ccf)
    wa = [A(R3 + i * 16384, 16384, BF16).rearrange("p (c n) -> p c n", c=16) for i in range(2)]
    mc_ps = bank(7, 96)
    colpieces = [0, 1, 2, 3, 4, 5, 6, 7, 12, 13, 14, 15, 16, 17, 18, 19]
    wa_k = [0]

    def ada_piece(pj):
        k = wa_k[0]
        wa_k[0] += 1
        w = wa[k % 2]
        P.dma(w, wada_d[:, pj * 512:(pj + 1) * 512].rearrange("(c p) n -> p c n", p=128), q='pool')
        for jj in range(4):
            col = pj * 4 + jj
            for fc in range(16):
                P.matmul(mc_ps[:, col:col + 1], w[:, fc, jj * 128:(jj + 1) * 128], cact[:, fc:fc + 1],
                         start=(k == 0 and jj == 0 and fc == 0), stop=True, skip_group_check=True)
    for pj in range(8):
        ada_piece(pj)
    P.tt(modcol[:, 0:32], mc_ps[:, 0:32], cols[:, C_BADA:C_BADA + 32], ALU.add)
    P.stt(s1col, modcol[:, 16:32], 1.0, cols[:, C_GPM:C_GPM + 16], ALU.add, ALU.mult)
    P.copy(sh1col, modcol[:, 0:16])
    late_pieces = [12, 13, 14, 15, 16, 17, 18, 19]

    def norm_transpose(xt_tile, dstT, tcol, scol, shcol, tmp_xn, junk, k):
        ss = stat[:, k % 8:k % 8 + 1]
        P.act(junk, xt_tile, AF.Square, accum_out=ss)
        rs = stat[:, 8 + k % 8:9 + k % 8]
        P.ts(rs, ss, 1.0 / D, 1e-6, ALU.mult, ALU.add)
        P.act(rs, rs, AF.Sqrt)
        P.op('dve', lambda e: e.reciprocal(rs, rs), outs=[rs], ins=[rs])
        P.ts(tmp_xn, xt_tile, rs, None, ALU.mult)
        for q4 in range(4):
            b = bank((k * 4 + q4) % 2)
            for j in range(4):
                fc = q4 * 4 + j
                P.transpose(b[:, j * 128:(j + 1) * 128], tmp_xn[:, fc * 128:(fc + 1) * 128], ident)
            for j in range(4):
                fc = q4 * 4 + j
                if (k * 4 + q4) % 2 == 0:
                    P.act(dstT[:, fc, tcol:tcol + 128], b[:, j * 128:(j + 1) * 128], AF.Identity,
                          scale=scol[:, fc:fc + 1], bias=shcol[:, fc:fc + 1])
                else:
                    P.ts(dstT[:, fc, tcol:tcol + 128], b[:, j * 128:(j + 1) * 128],
                         scol[:, fc:fc + 1], shcol[:, fc:fc + 1], ALU.mult, ALU.add)

    xts = [A(R3 + 32768 + i * 8192, 8192, F32) for i in range(2)]
    xns = [A(R3 + 49152 + i * 8192, 8192, F32) for i in range(2)]
    junkb = A(R3 + 65536, 4096, BF16)
    for tt in range(NT):
        xt = xts[tt % 2]
        P.dma(xt, x_d[tt * 128:(tt + 1) * 128, :])
        norm_transpose(xt, hT, tt * 128, s1col, sh1col, xns[tt % 2], junkb, tt)
        if tt % 2 == 1 and late_pieces:
            ada_piece(late_pieces.pop(0))
    while late_pieces:
        ada_piece(late_pieces.pop(0))
    P.tt(modcol[:, 48:80], mc_ps[:, 48:80], cols[:, C_BADA + 48:C_BADA + 80], ALU.add)
    P.stt(s2col, modcol[:, 64:80], 1.0, cols[:, C_GPL:C_GPL + 16], ALU.add, ALU.mult)
    P.copy(sh2col, modcol[:, 48:64])
    if dbg and stop == 1:
        P.dma(dbg_d[:, 0:2048].bitcast(BF16), hT[:, 0:2, :].rearrange("p c t -> p (c t)"))
        P.dma(dbg_d[:, 4096:4128], modcol[:, 0:32])
        P.dma(dbg_d[:, 4144:4176], modcol[:, 48:80])


    def proj_fm(wpiece, evac):
        for r in range(4):
            b = bank(2 + (proj_fm.cnt % 4))
            proj_fm.cnt += 1
            for fc in range(16):
                P.matmul(b, wpiece[:, fc, :], hT[:, fc, r * 512:(r + 1) * 512], start=(fc == 0), stop=(fc == 15))
            evac(r, b)
    proj_fm.cnt = 0

    def load_wpiece(dst, col0, ncol=128, dup64=False):
        if dup64:
            src = win_d[:, col0:col0 + 64].rearrange("(c p) n -> p c n", p=128)
            P.dma(dst[:, :, 0:64], src, q='pool')
            P.dma(dst[:, :, 64:128], src, q='pool')
        else:
            P.dma(dst[:, :, 0:ncol], win_d[:, col0:col0 + ncol].rearrange("(c p) n -> p c n", p=128), q='pool')

    conv = {}
    jobs = []
    if stop >= 5:
        for cq in range(4):
            for c4 in range(4):
                idx = cq * 4 + c4
                jobs.append((('wo', idx), wos[idx].rearrange("p (c n) -> p c n", c=4),
                             wout_d[c4 * 512:(c4 + 1) * 512, cq * 512:(cq + 1) * 512].rearrange("(c p) n -> p c n", p=128)))
        for e8 in range(8):
            for f8 in range(8):
                ffc = e8 * 8 + f8
                jobs.append((('w1', ffc), w1s[ffc].rearrange("p (c n) -> p c n", c=16),
                             wf1_d[:, ffc * 128:(ffc + 1) * 128].rearrange("(c p) n -> p c n", p=128)))
            for fo in range(4):
                for f4 in range(2):
                    idx = (e8 * 4 + fo) * 2 + f4
                    r0 = e8 * 1024 + f4 * 512
                    jobs.append((('w2', idx), w2s[idx].rearrange("p (c n) -> p c n", c=4),
                                 wf2_d[r0:r0 + 512, fo * 512:(fo + 1) * 512].rearrange("(c p) n -> p c n", p=128)))
    jobs.reverse()

    def bg_issue(n=3):
        for _ in range(n):
            if not jobs:
                return
            key, dst, src = jobs.pop()
            conv[key] = P.dma(dst, src, q='pool', chain='bg')
    P.bg_hook = bg_issue

    if stop >= 2:
        T = [A(R3 + i * 8192, 8192, F32) for i in range(7)]
        xrp = A(R3 + 57344, 8192 + 16, F32)
        xcb = A(R3 + 65600, 4096, BF16)
        ssum = A(R3 + 69696, 8192, F32)
        wxy = [A(R3 + 77888 + i * 4096, 4096, BF16).rearrange("p (c n) -> p c n", c=16) for i in range(4)]
        wra = A(R3 + 94272, 2048, BF16).rearrange("p (n j) -> p n j", n=8)
        wrx = A(R3 + 96320, 2048, BF16).rearrange("p (n j) -> p n j", n=8)
        P.dma(wra, wra_d.rearrange("n i j -> i n j"), q='pool')
        P.dma(wrx, wrx_d.rearrange("n i j -> i n j"), q='pool')
        lam = cols[:, C_LAM:C_LAM + 8]
        P.act(cA, lam, AF.Exp, scale=-1.0)
        P.act(cA, cA, AF.Ln, bias=1.0)
        P.ts(cA2, cA, -16.0, None, ALU.mult)
        P.ts(cA, cA, -8.0, None, ALU.mult)
        xrp2 = [xrp, A(R3 + 98368, 8192 + 16, F32)]
        for xp_ in xrp2:
            P.memset(xp_[:, 0:4], 0.0)

        def xr_proj(nb):
            wx = wxy[(2 * nb) % 4]
            load_wpiece(wx, XRc + nb * 128)
            dst = xrp2[nb % 2][:, 4:4 + S]
            proj_fm(wx, lambda r, b: P.copy(dst[:, r * 512:(r + 1) * 512], b, eng='act'))
        xr_proj(0)
        for nb in range(8):
            xrp = xrp2[nb % 2]
            wy = wxy[(2 * nb + 1) % 4]
            load_wpiece(wy, YRc + nb * 128)
            if nb + 1 < 8:
                xr_proj(nb + 1)
            xc = T[0]
            cw = lambda k: cols[:, C_CW + nb * 4 + k:C_CW + nb * 4 + k + 1]
            P.ts(xc, xrp[:, 4:4 + S], cw(3), cols[:, C_CB + nb:C_CB + nb + 1], ALU.mult, ALU.add)
            for k in (2, 1, 0):
                P.stt(xc, xrp[:, 1 + k:1 + k + S], cw(k), xc, ALU.mult, ALU.add)
            P.copy(xcb, xc)
            rg, ig = T[1], T[2]
            for (wr, dst, bc) in ((wra, rg, C_BA), (wrx, ig, C_BX)):
                for r in range(4):
                    b = bank(2 + (proj_fm.cnt % 4))
                    proj_fm.cnt += 1
                    P.matmul(b, wr[:, nb, :], xcb[:, r * 512:(r + 1) * 512])
                    P.act(dst[:, r * 512:(r + 1) * 512], b, AF.Sigmoid, bias=cols[:, bc + nb:bc + nb + 1])
            a_t, a2 = T[3], T[4]
            P.act(a_t, rg, AF.Exp, scale=cA[:, nb:nb + 1])
            P.act(a2, rg, AF.Exp, scale=cA2[:, nb:nb + 1])
            P.ts(a2, a2, -1.0, 1.0, ALU.mult, ALU.add)
            P.act(a2, a2, AF.Sqrt)
            P.tt(ig, ig, xc, ALU.mult)
            P.tt(ig, ig, a2, ALU.mult)
            hh = T[5]
            P.op('dve', lambda e, hh=hh, a_t=a_t, ig=ig: e.tensor_tensor_scan(hh, a_t, ig, 0.0, ALU.mult, ALU.add),
                 outs=[hh], ins=[a_t, ig])
            gy = T[6]
            proj_fm(wy, lambda r, b: P.act(gy[:, r * 512:(r + 1) * 512], b, AF.Gelu_apprx_tanh))
            P.tt(gy, gy, hh, ALU.mult)
            P.ts(orT[:, nb, :], gy, cols[:, C_GRNN + nb:C_GRNN + nb + 1], None, ALU.mult)
            sq = T[1]
            P.act(sq, gy, AF.Square)
            for r in range(4):
                b = bank(2 + (proj_fm.cnt % 4))
                proj_fm.cnt += 1
                P.matmul(b, onesf, sq[:, r * 512:(r + 1) * 512])
                if nb == 0:
                    P.copy(ssum[:, r * 512:(r + 1) * 512], b)
                else:
                    P.tt(ssum[:, r * 512:(r + 1) * 512], ssum[:, r * 512:(r + 1) * 512], b, ALU.add)
        P.ts(ssum, ssum, 1.0 / 1024, 1e-6, ALU.mult, ALU.add)
        P.act(ssum, ssum, AF.Sqrt)
        P.op('dve', lambda e: e.reciprocal(ssum, ssum), outs=[ssum], ins=[ssum])
        for nb in range(8):
            P.tt(orT[:, nb, :], orT[:, nb, :], ssum, ALU.mult)
        if dbg and stop == 2:
            P.dma(dbg_d[:, 0:2048].bitcast(BF16), orT[:, 0:2, :].rearrange("p c t -> p (c t)"))

    if stop >= 3:
        QT = A(R3, 32768, BF16).rearrange("p (c t) -> p c t", c=8)
        KcT = A(R3, 16384, BF16).rearrange("p (g t) -> p g t", g=4)
        VcT = A(R3 + 16384, 16384, BF16).rearrange("p (g t) -> p g t", g=4)
        KsT = A(R3 + 32768, 16384, BF16).rearrange("p (g t) -> p g t", g=4)
        KwT = A(R3 + 49152, 16384, BF16).rearrange("p (g t) -> p g t", g=4)
        Vs = A(R3 + 65536, 8320, BF16).rearrange("p (t g d) -> p t g d", t=16, g=4)
        Vw = A(R3 + 73984, 8320, BF16).rearrange("p (t g d) -> p t g d", t=16, g=4)
        G = A(R3 + 82432, 3072, F32).rearrange("p (t h) -> p t h", t=16)
        KcmpT = A(R3 + 85504, 1024, BF16).rearrange("p (g n) -> p g n", g=4)
        Vcmp = A(R3 + 86528, 800, BF16).rearrange("p (g d) -> p g d", g=4)
        wr3 = [A(R3 + 87360 + i * 4096, 4096, BF16).rearrange("p (c n) -> p c n", c=16) for i in range(3)]
        VT = A(R3 + 99648, 4096, BF16)
        w1k = A(R3 + 32768, 8192, BF16).rearrange("p (l j) -> p l j", l=16)
        w1v = A(R3 + 40960, 8192, BF16).rearrange("p (l j) -> p l j", l=16)
        w2kd = A(R3 + 49152, 512, BF16).rearrange("p (c n) -> p c n", c=2)
        w2vv = A(R3 + 49664, 256, BF16).rearrange("p (c n) -> p c n", c=2)
        pekb = A(R3 + 49920, 32, BF16)
        pevb = A(R3 + 49952, 32, BF16)
        hidT = A(R3 + 50176, 512, BF16).rearrange("p (c n) -> p c n", c=2)
        P.dma(w1k, w1k_d.rearrange("(l p) j -> p l j", p=128), q='pool')
        P.dma(w1v, w1v_d.rearrange("(l p) j -> p l j", p=128), q='pool')
        P.dma(w2kd[:, :, 0:64], w2k_d.rearrange("(c p) n -> p c n", p=128), q='pool')
        P.dma(w2kd[:, :, 64:128], w2k_d.rearrange("(c p) n -> p c n", p=128), q='pool')
        P.dma(w2vv, w2v_d.rearrange("(c p) n -> p c n", p=128), q='pool')
        P.copy(pekb, cols[:, C_PEK:C_PEK + 16])
        P.copy(pevb, cols[:, C_PEV:C_PEV + 16])
        wi = [0]

        def nextw():
            w = wr3[wi[0] % 3]
            wi[0] += 1
            return w

        def evac_dup2(dst, g):
            def f(r, b):
                P.copy(dst[0:64, g, r * 512:(r + 1) * 512], b[0:64, :], eng='act')
                if r == 0:
                    P.copy(dst[64:128, g, 0:511], b[64:128, 1:512])
                else:
                    P.copy(dst[64:128, g, r * 512 - 1:r * 512 + 511], b[64:128, :])
            return f
        for (dst, c0) in ((KcT, KCc), (VcT, VCc)):
            for g in range(4):
                w = nextw()
                load_wpiece(w, c0 + g * 64, dup64=True)
                proj_fm(w, evac_dup2(dst, g))
        for (w1, peb, pb) in ((w1k, pekb, pbk), (w1v, pevb, pbv)):
            b = bank(7, 96)
            for jc in range(2):
                for lp in range(16):
                    P.matmul(b[:, jc:jc + 1], w1[:, lp, jc * 128:(jc + 1) * 128], peb[:, lp:lp + 1],
                             start=(jc == 0 and lp == 0), stop=True, skip_group_check=True)
            P.copy(pb, b[:, 0:2])
        P.memset(Vcmp, 0.0)
        P.memset(KcmpT, 0.0)
        P.memset(Vcmp[:, :, 64:65], 1.0)
        for g in range(4):
            P.dma(Vcmp[:, g, 65:97], kc_d[:, K_W:K_W + 32], q='pool')
        for (src, w1, pb, isk) in ((KcT, w1k, pbk, True), (VcT, w1v, pbv, False)):
            for g in range(4):
                for jc in range(2):
                    b = bank(2 + (proj_fm.cnt % 4))
                    proj_fm.cnt += 1
                    for lp in range(16):
                        P.matmul(b[:, 0:127], w1[:, lp, jc * 128:(jc + 1) * 128],
                                 src[:, g, 2 * lp:2 * lp + 16 * 126 + 1:16], start=(lp == 0), stop=(lp == 15))
                    P.act(hidT[:, jc, 0:127], b[:, 0:127], AF.Gelu_apprx_tanh, bias=pb[:, jc:jc + 1])
                b = bank(2 + (proj_fm.cnt % 4))
                proj_fm.cnt += 1
                if isk:
                    for jc in range(2):
                        P.matmul(b[:, 0:127], w2kd[:, jc, :], hidT[:, jc, 0:127], start=(jc == 0), stop=(jc == 1))
                    P.copy(KcmpT[:, g, 0:127], b[:, 0:127])
                else:
                    for jc in range(2):
                        P.matmul(b[0:127, 0:64], hidT[:, jc, 0:127], w2vv[:, jc, :], start=(jc == 0), stop=(jc == 1))
                    P.copy(Vcmp[0:127, g, 0:64], b[0:127, 0:64])
        for (dst, c0) in ((KsT, KSc), (KwT, KWc)):
            for g in range(4):
                w = nextw()
                load_wpiece(w, c0 + g * 64, dup64=True)

                def f(r, b, dst=dst, g=g):
                    P.copy(dst[:, g, r * 512:(r + 1) * 512], b, eng=('act' if r % 2 else 'dve'))
                proj_fm(w, f)
        P.memset(Vs[:, :, :, 64:65], 1.0)
        P.memset(Vw[:, :, :, 64:65], 1.0)
        for (dst, c0) in ((Vs, VSc), (Vw, VWc)):
            for ch in range(2):
                w = nextw()
                load_wpiece(w, c0 + ch * 128)
                proj_fm(w, lambda r, b: P.copy(VT[:, r * 512:(r + 1) * 512], b, eng='act'))
                for tt in range(NT):
                    b = bank(tt % 2, 128, BF16)
                    P.transpose(b, VT[:, tt * 128:(tt + 1) * 128], identb)
                    P.copy(dst[:, tt, 2 * ch:2 * ch + 2, 0:64], b.rearrange("p (g d) -> p g d", g=2))
        w = nextw()
        load_wpiece(w, GLc, ncol=48)
        GT = A(R3, 8192, F32)
        for r in range(4):
            b = bank(2 + (proj_fm.cnt % 4))
            proj_fm.cnt += 1
            for fc in range(16):
                P.matmul(b[0:48, :], w[:, fc, 0:48], hT[:, fc, r * 512:(r + 1) * 512], start=(fc == 0), stop=(fc == 15))
            P.act(GT[0:48, r * 512:(r + 1) * 512], b[0:48, :], AF.Sigmoid)
        for tt in range(NT):
            b = bank(tt % 2, 48)
            P.transpose(b, GT[0:48, tt * 128:(tt + 1) * 128], ident[0:48, 0:48])
            P.copy(G[:, tt, :], b)
        for c in range(8):
            w = nextw()
            load_wpiece(w, QC + c * 128)

            def f(r, b, c=c):
                if r % 2:
                    P.act(QT[:, c, r * 512:(r + 1) * 512], b, AF.Identity, scale=0.125)
                else:
                    P.ts(QT[:, c, r * 512:(r + 1) * 512], b, 0.125, None, ALU.mult)
            proj_fm(w, f)
        if dbg and stop == 3:
            P.dma(dbg_d[:, 0:1024].bitcast(BF16), QT[:, 0, :])
            P.dma(dbg_d[:, 1024:2048].bitcast(BF16), KsT[:, 0, :])
            P.dma(dbg_d[:, 2048:2048 + 4160 // 2].bitcast(BF16), Vs.rearrange("p t g d -> p (t g d)"))
            P.dma(dbg_d[:, 4200:4200 + 768], G.rearrange("p t h -> p (t h)"))
            P.dma(dbg_d[:, 5000:5256].bitcast(BF16), KcmpT.rearrange("p g n -> p (g n)"))
            P.dma(dbg_d[:, 5300:5500].bitcast(BF16), Vcmp.rearrange("p g d -> p (g d)"))

    if stop >= 4:
        oaT = A(R1, 32768, BF16).rearrange("p (c t) -> p c t", c=8)
        B0 = R1 + 32768
        maskc = A(B0, 4096, BF16)
        Eexp = A(B0 + 4096, 4096, BF16).rearrange("p (k n) -> p k n", k=16)
        Am = A(B0 + 8192, 2048, F32).rearrange("p (t j) -> p t j", t=16)
        Bm = A(B0 + 10240, 2048, F32).rearrange("p (t j) -> p t j", t=16)
        caus4 = A(B0 + 12288, 4096, BF16).rearrange("p (o q) -> p o q", o=4)
        wmask = A(B0 + 16384, 8192, BF16).rearrange("p (o q) -> p o q", o=8)
        negmT = A(B0 + 24576, 4096, BF16).rearrange("p (g q) -> p g q", g=4)
        otmp = A(B0 + 28672, 1024, F32)
        sm = A(B0 + 29696, 1024, F32)
        oacc = [A(R3 + 87360 + i * 4096, 4096, F32) for i in range(4)]
        PT = [A(R3 + 103744 + i * 1024, 1024, BF16) for i in range(5)]
        onrm = A(R3 + 103744, 4096, F32)
        P.dma(maskc, kc_d[:, K_MASKC:K_MASKC + 2048], q='pool')
        P.dma(Eexp.rearrange("p k n -> p (k n)"), kc_d[:, K_E:K_E + 2048], q='pool')
        P.dma(Am.rearrange("p t j -> p (t j)"), kc_d[:, K_A:K_A + 512])
        P.dma(Bm.rearrange("p t j -> p (t j)"), kc_d[:, K_B:K_B + 512])
        P.dma(caus4.rearrange("p o q -> p (o q)"), kc_d[:, K_CAUS4:K_CAUS4 + 2048], q='pool')
        P.dma(wmask.rearrange("p o q -> p (o q)"), kc_d[:, K_WM:K_WM + 4096], q='pool')
        pt_i = [0]
        P.memset(negmT, 0.0)
        PVB = [bank(4 + j) for j in range(4)]
        PT = PT + [A(B0 + 30720 + 2048 - 1024, 1024, BF16)]
        NPT = len(PT)

        def score_one(KT, kcols, masks, QB, g, h, jv):
            q0, q1 = jv[0] * 128, (jv[-1] + 1) * 128
            bk = bank(h)
            first = True
            for (ml, mr) in masks:
                P.matmul(bk[:, q0:q1], ml, mr[:, q0:q1], start=first, stop=True, skip_group_check=True)
                first = False
            hh = 4 * g + h
            c, base = hh // 2, 64 * (hh % 2)
            P.matmul(bk[:, q0:q1], KT[base:base + 64, g, kcols], QT[base:base + 64, c, QB * 512 + q0:QB * 512 + q1],
                     start=first, stop=True, skip_group_check=True)
            pt = PT[pt_i[0] % NPT]
            pt_i[0] += 1
            P.act(pt[:, q0:q1], bk[:, q0:q1], AF.Exp)
            return pt

        def run_branch(iters, QB, g, width):
            started = [False] * 4
            pending = [None]

            def do_pv():
                if pending[0] is None:
                    return
                pts, hs, V, jv = pending[0]
                for j in jv:
                    pv = PVB[j][:, 0:4 * width].rearrange("p (h d) -> p h d", h=4)
                    for pt, h in zip(pts, hs):
                        P.matmul(pv[:, h, :], pt[:, j * 128:(j + 1) * 128], V,
                                 start=(not started[j]), stop=True, skip_group_check=True)
                        started[j] = True
                pending[0] = None
            for (KT, kcols, masks, V, jv) in iters:
                for hs in ((0, 1), (2, 3)):
                    pts = [score_one(KT, kcols, masks, QB, g, h, jv) for h in hs]
                    do_pv()
                    pending[0] = (pts, hs, V, jv)
            do_pv()

        def finish_branch(pv, qt, g, br, oa, first, den, wg):
            P.ts(den, pv[:, :, 64], 1e-30, None, ALU.max)
            P.op('dve', lambda e: e.reciprocal(den, den), outs=[den], ins=[den])
            P.tt(wg, den, G[:, qt, 12 * g + br:12 * g + br + 10:3], ALU.mult)
            dst = oa.rearrange("p (h d) -> p h d", h=16)[:, 4 * g:4 * g + 4, :]
            wgb = wg.unsqueeze(2).broadcast_to([128, 4, 64])
            if first:
                P.tt(dst, pv[:, :, 0:64], wgb, ALU.mult)
            else:
                t3 = otmp.rearrange("p (h d) -> p h d", h=4)
                P.tt(t3, pv[:, :, 0:64], wgb, ALU.mult)
                P.tt(dst, dst, t3, ALU.add)

        den_c = lambda j: sm[:, 4 * j:4 * j + 4]

        def cmp_part(QB, g):
            run_branch([(KcmpT, slice(0, 128), [(identb, maskc[:, QB * 512:(QB + 1) * 512])],
                         Vcmp[:, g, 0:97], list(range(4)))], QB, g, 97)

        def cmp_finish(QB, g):
            for j in range(4):
                qt = 4 * QB + j
                pvc = PVB[j][:, 0:388].rearrange("p (h d) -> p h d", h=4)
                finish_branch(pvc, qt, g, 0, oacc[j], True, den_c(j), sm[:, 16:20])
                imp = sm[:, 64 + 32 * j:96 + 32 * j]
                P.ts(imp, pvc[:, 0, 65:97], den_c(j)[:, 0:1], None, ALU.mult)
                for h in range(1, 4):
                    P.stt(imp, pvc[:, h, 65:97], den_c(j)[:, h:h + 1], imp, ALU.mult, ALU.add)
            for j in range(4):
                qt = 4 * QB + j
                imp = sm[:, 64 + 32 * j:96 + 32 * j]
                P.tt(imp, imp, Am[:, qt, :], ALU.mult)
                P.tt(imp, imp, Bm[:, qt, :], ALU.add)
                top8 = sm[:, 24 + 8 * j:32 + 8 * j]
                P.op('dve', lambda e, top8=top8, imp=imp: e.max(top8, imp), outs=[top8], ins=[imp])
                P.ts(imp, imp, top8[:, 7:8], None, ALU.is_ge)
                P.ts(imp, imp, -NEGV, NEGV, ALU.mult, ALU.add)

        def sel_masks_T(QB, g):
            for j in range(4):
                negm = sm[:, 64 + 32 * j:96 + 32 * j]
                bt = bank(j)[:, 0:128]
                P.transpose(bt[0:32, :], negm, ident)
                P.copy(negmT[0:32, g, j * 128:(j + 1) * 128], bt[0:32, :], eng='act')

        def win_part(QB, g):
            iters = []
            for o in range(8):
                kt = 4 * QB - 4 + o
                if kt < 0:
                    continue
                jv = [j for j in range(4) if 0 <= j + 4 - o <= 4]
                iters.append((KwT, slice(kt * 128, (kt + 1) * 128), [(identb, wmask[:, o, :])], Vw[:, kt, g, :], jv))
            run_branch(iters, QB, g, 65)
            for j in range(4):
                pvw = PVB[j][:, 0:260].rearrange("p (h d) -> p h d", h=4)
                finish_branch(pvw, 4 * QB + j, g, 2, oacc[j], False, sm[:, 208:212], sm[:, 212:216])

        def sel_part(QB, g):
            iters = []
            for kt in range(4 * QB + 4):
                masks = [(Eexp[:, kt, :], negmT[:, g, :])]
                if kt >= 4 * QB:
                    masks.append((identb, caus4[:, kt - 4 * QB, :]))
                jv = [j for j in range(4) if 4 * QB + j >= kt]
                iters.append((KsT, slice(kt * 128, (kt + 1) * 128), masks, Vs[:, kt, g, :], jv))
            run_branch(iters, QB, g, 65)
            for j in range(4):
                pvs = PVB[j][:, 0:260].rearrange("p (h d) -> p h d", h=4)
                finish_branch(pvs, 4 * QB + j, g, 1, oacc[j], False, sm[:, 216:220], sm[:, 220:224])

        for QB in range(4):
            for g in range(4):
                cmp_part(QB, g)
                cmp_finish(QB, g)
                win_part(QB, g)
                sel_masks_T(QB, g)
                sel_part(QB, g)
            for j in range(4):
                qt = 4 * QB + j
                oa = oacc[j]
                ss = sm[:, 228 + j:229 + j]
                P.act(onrm, oa, AF.Square, accum_out=ss)
                P.ts(ss, ss, 1.0 / 1024, 1e-6, ALU.mult, ALU.add)
                P.act(ss, ss, AF.Sqrt)
                P.op('dve', lambda e, ss=ss: e.reciprocal(ss, ss), outs=[ss], ins=[ss])
                P.ts(onrm, oa, ss, None, ALU.mult)
                for half in range(2):
                    b = bank(half)
                    for jj in range(4):
                        c = half * 4 + jj
                        P.transpose(b[:, jj * 128:(jj + 1) * 128], onrm[:, c * 128:(c + 1) * 128], ident)
                    for jj in range(4):
                        c = half * 4 + jj
                        P.ts(oaT[:, c, qt * 128:(qt + 1) * 128], b[:, jj * 128:(jj + 1) * 128],
                             cols[:, C_GATT + c:C_GATT + c + 1], None, ALU.mult)
        if dbg and stop == 4:
            P.dma(dbg_d[:, 0:4096].bitcast(BF16), oaT[:, 0:4, :].rearrange("p c t -> p (c t)"))

    if stop >= 5:
        E0 = R1 + 32768
        GM1 = A(E0, 8192, F32)
        GM2 = A(E0 + 8192, 8192, F32)
        P.bg_hook = None
        bg_issue(10 ** 6)
        r42 = [A(E0 + 16384 + i * 4096, 4096, BF16).rearrange("p (c n) -> p c n", c=4) for i in range(4)]
        x1 = [A(R3 + i * 8192, 8192, F32) for i in range(4)]
        fb = [A(R3 + 32768 + i * 8192, 8192, F32) for i in range(4)]
        h2T = A(R3 + 65536, 16384, BF16).rearrange("p (c t) -> p c t", c=16)
        uT = A(R3 + 81920, 8192, BF16).rearrange("p (c t) -> p c t", c=8)
        w1_r = [A(R3 + 90112 + i * 4096, 4096, BF16).rearrange("p (c n) -> p c n", c=16) for i in range(4)]
        rtmp = A(R3 + 106496, 2048, F32)
        junk2 = A(R3 + 81920, 4096, BF16)
        crep = A(R3 + 104448, 4096, BF16).rearrange("p (c m) -> p c m", c=16)
        P.copy(crep, cact.unsqueeze(2).broadcast_to([128, 16, 128]))
        wa2 = [A(R3 + i * 16384, 16384, BF16).rearrange("p (c n) -> p c n", c=16) for i in range(2)]
        brow = A(R3 + 32768, 2048, F32)
        grow = A(R3 + 34816, 2048, F32)
        for k, pj in enumerate([8, 9, 10, 11, 20, 21, 22, 23]):
            w = wa2[k % 2]
            P.dma(w, wada_d[:, pj * 512:(pj + 1) * 512].rearrange("(c p) n -> p c n", p=128), q='pool')
            P.dma(brow, bgt_d[:, k * 512:(k + 1) * 512])
            P.dma(grow, gpost_d[:, k * 512:(k + 1) * 512])
            b = bank(k % 2)
            for fc in range(16):
                P.matmul(b, crep[:, fc, :], w[:, fc, :], start=(fc == 0), stop=(fc == 15))
            dst = (GM1 if k < 4 else GM2)[:, (k % 4) * 512:(k % 4 + 1) * 512]
            P.tt(dst, b, brow, ALU.add)
            P.tt(dst, dst, grow, ALU.mult)
        oT = lambda c: (oaT[:, c, :] if c < 8 else orT[:, c - 8, :])
        wo_i, w1_i, w2_i, fi = [0], [0], [0], [0]
        for tb in range(4):
            t0 = tb * 512
            for tt in range(4):
                P.dma(x1[tt], x_d[t0 + tt * 128:t0 + (tt + 1) * 128, :])
            for cq in range(4):
                pb = [bank(4 + tt) for tt in range(4)]
                for c4 in range(4):
                    w = r42[wo_i[0] % 4]
                    wo_i[0] += 1
                    P.dma(w, wos[cq * 4 + c4].rearrange("p (c n) -> p c n", c=4), deps=[conv[('wo', cq * 4 + c4)]])
                    for cc in range(4):
                        c = c4 * 4 + cc
                        for tt in range(4):
                            P.matmul(pb[tt], oT(c)[:, t0 + tt * 128:t0 + (tt + 1) * 128], w[:, cc, :],
                                     start=(c == 0), stop=(c == 15))
                for tt in range(4):
                    P.copy(fb[tt][:, cq * 512:(cq + 1) * 512], pb[tt], eng=('act' if tt % 2 else 'dve'))
            for tt in range(4):
                ss = stat[:, 16 + tt:17 + tt]
                P.act(junk2, fb[tt], AF.Square, accum_out=ss)
                P.ts(ss, ss, 1.0 / D, 1e-6, ALU.mult, ALU.add)
                P.act(ss, ss, AF.Sqrt)
                P.op('dve', lambda e, ss=ss: e.reciprocal(ss, ss), outs=[ss], ins=[ss])
                P.stt(fb[tt], fb[tt], ss, GM1, ALU.mult, ALU.mult)
                P.tt(x1[tt], x1[tt], fb[tt], ALU.add)
                norm_transpose(x1[tt], h2T, tt * 128, s2col, sh2col, fb[tt], junk2, tt)
            for e8 in range(8):
                for f8 in range(8):
                    ffc = e8 * 8 + f8
                    w = w1_r[w1_i[0] % 4]
                    w1_i[0] += 1
                    P.dma(w, w1s[ffc].rearrange("p (c n) -> p c n", c=16), deps=[conv[('w1', ffc)]])
                    b = bank(fi[0] % 4)
                    fi[0] += 1
                    for fc in range(16):
                        P.matmul(b, w[:, fc, :], h2T[:, fc, :], start=(fc == 0), stop=(fc == 15))
                    P.act(rtmp, b, AF.Relu)
                    P.tt(uT[:, f8, :], rtmp, rtmp, ALU.mult)
                for fo in range(4):
                    pb = [bank(4 + tt) for tt in range(4)]
                    for f4 in range(2):
                        w = r42[wo_i[0] % 4]
                        wo_i[0] += 1
                        P.dma(w, w2s[(e8 * 4 + fo) * 2 + f4].rearrange("p (c n) -> p c n", c=4),
                              deps=[conv[('w2', (e8 * 4 + fo) * 2 + f4)]])
                        for cc in range(4):
                            f8 = f4 * 4 + cc
                            for tt in range(4):
                                P.matmul(pb[tt], uT[:, f8, tt * 128:(tt + 1) * 128], w[:, cc, :],
                                         start=(f8 == 0), stop=(f8 == 7))
                    for tt in range(4):
                        dst = fb[tt][:, fo * 512:(fo + 1) * 512]
                        if e8 == 0:
                            P.copy(dst, pb[tt], eng=('act' if tt % 2 else 'dve'))
                        else:
                            P.tt(dst, dst, pb[tt], ALU.add)
            for tt in range(4):
                ss = stat[:, 24 + tt:25 + tt]
                P.act(junk2, fb[tt], AF.Square, accum_out=ss)
                P.ts(ss, ss, 1.0 / D, 1e-6, ALU.mult, ALU.add)
                P.act(ss, ss, AF.Sqrt)
                P.op('dve', lambda e, ss=ss: e.reciprocal(ss, ss), outs=[ss], ins=[ss])
                P.stt(fb[tt], fb[tt], ss, GM2, ALU.mult, ALU.mult)
                P.tt(fb[tt], fb[tt], x1[tt], ALU.add)
                P.dma(out_d[t0 + tt * 128:t0 + (tt + 1) * 128, :], fb[tt], is_output=True)
    if stop < 5:
        P.dma(out_d[0:128, :], A(R3, 8192, F32), is_output=True)
    if dbg:
        P.out_dmas = [o for o in P.ops['sp'] if o.is_dma] + [o for o in P.out_dmas]
    P.emit()
    st.close()
    return nc, P


def host_consts():
    ident = np.eye(128, dtype=np.float32)
    kc = np.zeros((128, NKC), np.float32)
    key = np.arange(128)[:, None]
    q = np.arange(128)[None, :]
    cm = np.where(key > q, NEGV, 0.0)
    wm = np.where(key <= q, NEGV, 0.0)
    full = np.full((128, 128), NEGV)
    zero = np.zeros((128, 128))
    for o in range(4):
        kc[:, K_CAUS4 + o * 512:K_CAUS4 + (o + 1) * 512] = np.concatenate(
            [full if j < o else (cm if j == o else zero) for j in range(4)], 1)
    for o in range(8):
        tiles = []
        for j in range(4):
            dd = j + 4 - o
            tiles.append(full if (dd < 0 or dd > 4) else (cm if dd == 0 else (wm if dd == 4 else zero)))
        kc[:, K_WM + o * 512:K_WM + (o + 1) * 512] = np.concatenate(tiles, 1)
    n = np.arange(128)[:, None]
    pos = np.arange(2048)[None, :]
    kc[:, K_MASKC:K_MASKC + 2048] = np.where(16 * n + 31 <= pos, 0.0, NEGV)
    posq = np.arange(2048)
    blk = np.arange(32)[None, :]
    cur = (posq // 64)[:, None]
    forced = (blk == 0) | (blk == cur) | (blk == cur - 1)
    valid = blk * 64 <= posq[:, None]
    Am = np.where(forced | ~valid, 0.0, 1.0).astype(np.float32)
    Bm = np.where(forced, 1e9, np.where(valid, 0.0, -1e9)).astype(np.float32)
    kc[:, K_A:K_A + 512] = Am.reshape(16, 128, 32).transpose(1, 0, 2).reshape(128, 512)
    kc[:, K_B:K_B + 512] = Bm.reshape(16, 128, 32).transpose(1, 0, 2).reshape(128, 512)
    E = np.zeros((128, 16, 128), np.float32)
    for kt in range(16):
        for k in range(128):
            E[(kt * 128 + k) // 64, kt, k] = 1.0
    kc[:, K_E:K_E + 2048] = E.reshape(128, 2048)
    c0 = np.arange(127)[:, None] * 16
    s0 = np.arange(32)[None, :] * 64
    ov = np.minimum(c0 + 32, s0 + 64) - np.maximum(c0, s0)
    kc[0:127, K_W:K_W + 32] = np.clip(ov, 0, None).astype(np.float32) / 32
    return ident, kc


def make_in_maps(inp):
    f = lambda a: np.ascontiguousarray(np.asarray(a, dtype=np.float32))
    ident, kc = host_consts()
    col = lambda v: f(v).reshape(-1, 128).T
    b_ada = f(inp['b_ada'])[0]
    shared_cols = np.zeros((128, NCOLS), np.float32)
    shared_cols[:, C_BADA:C_BADA + 96] = col(b_ada)
    shared_cols[:, C_GPM:C_GPM + 16] = col(inp['g_pre_mix'][0])
    shared_cols[:, C_GPL:C_GPL + 16] = col(inp['g_pre_mlp'][0])
    cw = f(inp['conv_w'])[0]
    shared_cols[:, C_CW:C_CW + 32] = cw.T.reshape(8, 128, 4).transpose(1, 0, 2).reshape(128, 32)
    shared_cols[:, C_CB:C_CB + 8] = col(inp['conv_b'][0])
    shared_cols[:, C_BA:C_BA + 8] = col(inp['b_rg_a'][0])
    shared_cols[:, C_BX:C_BX + 8] = col(inp['b_rg_x'][0])
    shared_cols[:, C_LAM:C_LAM + 8] = col(inp['lru_lambda'][0])
    shared_cols[:, C_GRNN:C_GRNN + 8] = col(inp['g_grp_rnn'][0])
    shared_cols[:, C_GATT:C_GATT + 8] = col(inp['g_grp_att'][0])
    shared_cols[:, C_PEK:C_PEK + 16] = col(f(inp['cmp_pe_k'])[0].reshape(-1))
    shared_cols[:, C_PEV:C_PEV + 16] = col(f(inp['cmp_pe_v'])[0].reshape(-1))
    bgt = np.concatenate([b_ada[4096:6144], b_ada[10240:12288]])
    bgt_rows = np.ascontiguousarray(np.broadcast_to(bgt[None, :], (128, 4096)))
    gpost = np.concatenate([f(inp['g_post_mix'])[0], f(inp['g_post_mlp'])[0]])
    gpost_rows = np.ascontiguousarray(np.broadcast_to(gpost[None, :], (128, 4096)))
    shared = {
        "kconst": kc, "ident": ident, "w_ada": f(inp['w_ada'])[0], "bgt_rows": bgt_rows, "gpost_rows": gpost_rows,
        "w_in": f(inp['w_in'])[0], "cmp_w1_k": f(inp['cmp_w1_k'])[0], "cmp_w2_k": f(inp['cmp_w2_k'])[0],
        "cmp_w1_v": f(inp['cmp_w1_v'])[0], "cmp_w2_v": f(inp['cmp_w2_v'])[0],
        "w_rg_a": f(inp['w_rg_a'])[0], "w_rg_x": f(inp['w_rg_x'])[0], "w_out": f(inp['w_out'])[0],
        "w_ff1": f(inp['w_ff1'])[0], "w_ff2": f(inp['w_ff2'])[0],
    }
    x = f(inp['x'])
    c = f(inp['c'])
    maps = []
    for b in range(8):
        cols_b = shared_cols.copy()
        cols_b[:, C_C:C_C + 16] = col(c[b])
        m = dict(shared)
        m["x"] = x[b]
        m["cols"] = cols_b
        maps.append(m)
    return maps


def kernel(**inputs):
    nc = bass.Bass("TRN2", target_bir_lowering=False)
    build(nc)
    maps = make_in_maps(inputs)
    res = run_bass_kernel_spmd(nc, maps, core_ids=list(range(8)))
    return np.stack([np.asarray(r["out"], dtype=np.float32) for r in res.results], axis=0)
```
